# Optimizing a Trainium2 kernel written in Bass

```python
import jax, jax.numpy as jnp
from jax import lax
import numpy as np

D_MODEL = 1024
BATCH = 2
SEQ = 16384
DEPTH = 2

D_MIX = D_MODEL
EPS = 1e-6
NEG = -1e30
FORCED = 1e6

NSA_DH = 64
NSA_W = D_MIX // 2
NSA_HQ = NSA_W // NSA_DH
NSA_HKV = 2
NSA_G = NSA_HQ // NSA_HKV
KV_W = NSA_HKV * NSA_DH
CMP_BLOCK = 32
CMP_STRIDE = 16
CMP_HID = 128
SLC_BLOCK = 64
N_SEL = 16
WINDOW = 512
Q_BLOCK = 128

LRU_W = D_MIX // 4
LRU_HEADS = 8
LRU_BW = LRU_W // LRU_HEADS
LRU_CONV = 4
LRU_C = 8.0

CV_W = D_MIX // 4
CV_KERNEL = 31

D_FF = 4 * D_MODEL

OFF_Q = 0
OFF_KV = OFF_Q + NSA_W
OFF_GATE = OFF_KV + 6 * KV_W
OFF_LRU_X = OFF_GATE + 3 * NSA_HQ
OFF_LRU_G = OFF_LRU_X + LRU_W
OFF_CV = OFF_LRU_G + LRU_W
N_IN = OFF_CV + 2 * CV_W

kernel_name = "hymba_nsa_rglru_conformer_trunk"


def rms_norm(x, g):
    xf = x.astype(jnp.float32)
    y = xf * lax.rsqrt(jnp.mean(xf * xf, axis=-1, keepdims=True) + EPS)
    return (y * g.astype(jnp.float32)).astype(x.dtype)


def layer_norm(x, g, b):
    xf = x.astype(jnp.float32)
    mu = jnp.mean(xf, axis=-1, keepdims=True)
    var = jnp.mean(jnp.square(xf - mu), axis=-1, keepdims=True)
    y = (xf - mu) * lax.rsqrt(var + EPS)
    return (y * g.astype(jnp.float32) + b.astype(jnp.float32)).astype(x.dtype)


def causal_depthwise_conv(x, w, b):
    k = w.shape[0]
    y = lax.conv_general_dilated(
        x, w[:, None, :].astype(x.dtype), window_strides=(1,), padding=[(k - 1, 0)],
        dimension_numbers=('NWC', 'WIO', 'NWC'), feature_group_count=x.shape[-1])
    return y + b.astype(x.dtype)


def compress(kv, pos, w1, w2):
    b, s, h, dh = kv.shape
    c = kv.reshape(b, s // CMP_STRIDE, CMP_STRIDE, h, dh)
    blocks = jnp.concatenate([c[:, :-1], c[:, 1:]], axis=2)
    blocks = blocks + pos[None, None, :, None, :].astype(kv.dtype)
    nc = blocks.shape[1]
    flat = blocks.transpose(0, 1, 3, 2, 4).reshape(b, nc, h, CMP_BLOCK * dh)
    return jax.nn.gelu(flat @ w1) @ w2


def nsa_attention(q, kc, vc, ks, vs, kw, vw, gates):
    b, h, g, s, dh = q.shape
    nc = kc.shape[2]
    n_slc = s // SLC_BLOCK
    n_sel = min(N_SEL, n_slc)
    ratio = SLC_BLOCK // CMP_STRIDE
    scale = dh ** -0.5
    ks_b = ks.reshape(b, h, n_slc, SLC_BLOCK, dh)
    vs_b = vs.reshape(b, h, n_slc, SLC_BLOCK, dh)
    kw_p = jnp.pad(kw, ((0, 0), (0, 0), (WINDOW, 0), (0, 0)))
    vw_p = jnp.pad(vw, ((0, 0), (0, 0), (WINDOW, 0), (0, 0)))
    cmp_end = jnp.arange(nc) * CMP_STRIDE + CMP_BLOCK - 1
    blk = jnp.arange(n_slc)
    gather = jax.vmap(jax.vmap(lambda kb, ix: kb[ix]))

    def block_fn(i):
        start = i * Q_BLOCK
        t = start + jnp.arange(Q_BLOCK)
        qb = lax.dynamic_slice_in_dim(q, start, Q_BLOCK, axis=3)
        gb = lax.dynamic_slice_in_dim(gates, start, Q_BLOCK, axis=3).astype(jnp.float32)

        s_c = jnp.einsum('bhgqd,bhnd->bhgqn', qb, kc).astype(jnp.float32) * scale
        m_c = cmp_end[None, :] <= t[:, None]
        p_c = jax.nn.softmax(jnp.where(m_c, s_c, NEG), axis=-1) * m_c
        o_c = jnp.einsum('bhgqn,bhnd->bhgqd', p_c.astype(vc.dtype), vc)

        imp = p_c.sum(axis=2)
        imp = jnp.pad(imp, ((0, 0), (0, 0), (0, 0), (1, ratio)))
        imp_slc = (imp[..., :ratio * n_slc].reshape(b, h, Q_BLOCK, n_slc, ratio).sum(-1)
                   + imp[..., ratio::ratio])
        cur = t // SLC_BLOCK
        valid = blk[None, :] * SLC_BLOCK <= t[:, None]
        forced = ((blk[None, :] == 0) | (blk[None, :] == cur[:, None])
                  | (blk[None, :] == cur[:, None] - 1))
        score = jnp.where(valid, jnp.where(forced, FORCED, imp_slc), NEG)
        _, idx = lax.top_k(score, n_sel)

        k_sel = gather(ks_b, idx)
        v_sel = gather(vs_b, idx)
        s_s = jnp.einsum('bhgqd,bhqnld->bhgqnl', qb, k_sel).astype(jnp.float32) * scale
        pos = idx[..., None] * SLC_BLOCK + jnp.arange(SLC_BLOCK)
        m_s = (pos <= t[:, None, None])[:, :, None]
        s_s = jnp.where(m_s, s_s, NEG).reshape(b, h, g, Q_BLOCK, n_sel * SLC_BLOCK)
        p_s = jax.nn.softmax(s_s, axis=-1).reshape(b, h, g, Q_BLOCK, n_sel, SLC_BLOCK)
        o_s = jnp.einsum('bhgqnl,bhqnld->bhgqd', p_s.astype(v_sel.dtype), v_sel)

        kwb = lax.dynamic_slice_in_dim(kw_p, start, WINDOW + Q_BLOCK, axis=2)
        vwb = lax.dynamic_slice_in_dim(vw_p, start, WINDOW + Q_BLOCK, axis=2)
        s_w = jnp.einsum('bhgqd,bhkd->bhgqk', qb, kwb).astype(jnp.float32) * scale
        kpos = start - WINDOW + jnp.arange(WINDOW + Q_BLOCK)
        m_w = ((kpos[None, :] <= t[:, None]) & (kpos[None, :] > t[:, None] - WINDOW)
               & (kpos[None, :] >= 0))
        p_w = jax.nn.softmax(jnp.where(m_w, s_w, NEG), axis=-1)
        o_w = jnp.einsum('bhgqk,bhkd->bhgqd', p_w.astype(vwb.dtype), vwb)

        o = gb[..., 0:1] * o_c + gb[..., 1:2] * o_s + gb[..., 2:3] * o_w
        return o.astype(q.dtype)

    out = lax.map(block_fn, jnp.arange(s // Q_BLOCK))
    return out.transpose(1, 0, 4, 2, 3, 5).reshape(b, s, h * g * dh)


def rglru(xb, gb, conv_w, conv_b, wa, ba, wx, bx, lam):
    b, s, w = xb.shape
    xr = causal_depthwise_conv(xb, conv_w, conv_b)
    xh = xr.reshape(b, s, LRU_HEADS, LRU_BW)
    r = jax.nn.sigmoid(jnp.einsum('bshi,hij->bshj', xh, wa).reshape(b, s, w).astype(jnp.float32)
                       + ba.astype(jnp.float32))
    ig = jax.nn.sigmoid(jnp.einsum('bshi,hij->bshj', xh, wx).reshape(b, s, w).astype(jnp.float32)
                        + bx.astype(jnp.float32))
    log_a = -LRU_C * r * jax.nn.softplus(-lam.astype(jnp.float32))
    a = jnp.exp(log_a)
    mult = jnp.sqrt(-jnp.expm1(2.0 * log_a))
    u = xr.astype(jnp.float32) * ig * mult

    def combine(c1, c2):
        a1, b1 = c1
        a2, b2 = c2
        return a1 * a2, a2 * b1 + b2

    _, hseq = lax.associative_scan(combine, (a, u), axis=1)
    return (hseq * jax.nn.gelu(gb.astype(jnp.float32))).astype(xb.dtype)


def conformer_conv(u, dw_w, dw_b, ln_g, ln_b):
    a, gt = jnp.split(u, 2, axis=-1)
    y = a * jax.nn.sigmoid(gt)
    y = causal_depthwise_conv(y, dw_w, dw_b)
    y = layer_norm(y, ln_g, ln_b)
    return jax.nn.silu(y)


def setup_inputs(seed: int = 0) -> dict:
    key = jax.random.key(seed)
    ks = jax.random.split(key, 32)
    f32 = jnp.float32
    nrm = lambda k, shape, scale: jax.random.normal(k, shape, f32) * scale
    u = jax.random.uniform(ks[20], (DEPTH, LRU_W), f32, minval=0.9, maxval=0.999)
    p = u ** (1.0 / LRU_C)
    return {
        "x": jax.random.normal(ks[0], (BATCH, SEQ, D_MODEL), f32),
        "attn_norm": 1.0 + nrm(ks[1], (DEPTH, D_MODEL), 0.02),
        "w_in": nrm(ks[2], (DEPTH, D_MODEL, N_IN), D_MODEL ** -0.5),
        "q_norm": 1.0 + nrm(ks[3], (DEPTH, NSA_DH), 0.02),
        "k_norm": 1.0 + nrm(ks[4], (DEPTH, 3, NSA_DH), 0.02),
        "cmp_pos": nrm(ks[5], (DEPTH, 2, CMP_BLOCK, NSA_DH), 0.1),
        "cmp_w1": nrm(ks[6], (DEPTH, 2, CMP_BLOCK * NSA_DH, CMP_HID), (CMP_BLOCK * NSA_DH) ** -0.5),
        "cmp_w2": nrm(ks[7], (DEPTH, 2, CMP_HID, NSA_DH), CMP_HID ** -0.5),
        "lru_conv_w": nrm(ks[8], (DEPTH, LRU_CONV, LRU_W), LRU_CONV ** -0.5),
        "lru_conv_b": nrm(ks[9], (DEPTH, LRU_W), 0.02),
        "lru_wa": nrm(ks[10], (DEPTH, LRU_HEADS, LRU_BW, LRU_BW), LRU_BW ** -0.5),
        "lru_ba": nrm(ks[11], (DEPTH, LRU_W), 0.02),
        "lru_wx": nrm(ks[12], (DEPTH, LRU_HEADS, LRU_BW, LRU_BW), LRU_BW ** -0.5),
        "lru_bx": nrm(ks[13], (DEPTH, LRU_W), 0.02),
        "lru_lambda": jnp.log(p) - jnp.log1p(-p),
        "cv_dw_w": nrm(ks[14], (DEPTH, CV_KERNEL, CV_W), CV_KERNEL ** -0.5),
        "cv_dw_b": nrm(ks[15], (DEPTH, CV_W), 0.02),
        "cv_ln_g": 1.0 + nrm(ks[16], (DEPTH, CV_W), 0.02),
        "cv_ln_b": nrm(ks[17], (DEPTH, CV_W), 0.02),
        "out_norm": 1.0 + nrm(ks[18], (DEPTH, D_MIX), 0.02),
        "w_out": nrm(ks[19], (DEPTH, D_MIX, D_MODEL), (2.0 * D_MIX) ** -0.5),
        "mlp_norm": 1.0 + nrm(ks[21], (DEPTH, D_MODEL), 0.02),
        "mlp_w1": nrm(ks[22], (DEPTH, D_MODEL, D_FF), D_MODEL ** -0.5),
        "mlp_w2": nrm(ks[23], (DEPTH, D_FF, D_MODEL), (2.0 * D_FF) ** -0.5),
    }


def reference(x, attn_norm, w_in, q_norm, k_norm, cmp_pos, cmp_w1, cmp_w2,
              lru_conv_w, lru_conv_b, lru_wa, lru_ba, lru_wx, lru_bx, lru_lambda,
              cv_dw_w, cv_dw_b, cv_ln_g, cv_ln_b, out_norm, w_out,
              mlp_norm, mlp_w1, mlp_w2):
    b, s, _ = x.shape
    for l in range(DEPTH):
        hn = rms_norm(x, attn_norm[l])
        z = hn @ w_in[l]

        q = rms_norm(z[..., OFF_Q:OFF_KV].reshape(b, s, NSA_HKV, NSA_G, NSA_DH), q_norm[l])
        q = q.transpose(0, 2, 3, 1, 4)
        kv = z[..., OFF_KV:OFF_GATE].reshape(b, s, 6, NSA_HKV, NSA_DH)
        kc = rms_norm(compress(kv[:, :, 0], cmp_pos[l, 0], cmp_w1[l, 0], cmp_w2[l, 0]), k_norm[l, 0])
        vc = compress(kv[:, :, 1], cmp_pos[l, 1], cmp_w1[l, 1], cmp_w2[l, 1])
        k_s = rms_norm(kv[:, :, 2], k_norm[l, 1])
        k_w = rms_norm(kv[:, :, 4], k_norm[l, 2])
        tr = lambda a: a.transpose(0, 2, 1, 3)
        gates = jax.nn.sigmoid(z[..., OFF_GATE:OFF_LRU_X].reshape(b, s, NSA_HKV, NSA_G, 3))
        gates = gates.transpose(0, 2, 3, 1, 4)
        y_attn = nsa_attention(q, tr(kc), tr(vc), tr(k_s), tr(kv[:, :, 3]),
                               tr(k_w), tr(kv[:, :, 5]), gates)

        y_lru = rglru(z[..., OFF_LRU_X:OFF_LRU_G], z[..., OFF_LRU_G:OFF_CV],
                      lru_conv_w[l], lru_conv_b[l], lru_wa[l], lru_ba[l],
                      lru_wx[l], lru_bx[l], lru_lambda[l])

        y_cv = conformer_conv(z[..., OFF_CV:N_IN], cv_dw_w[l], cv_dw_b[l],
                              cv_ln_g[l], cv_ln_b[l])

        g_out = out_norm[l]
        y = jnp.concatenate([
            rms_norm(y_attn, g_out[:NSA_W]),
            rms_norm(y_lru, g_out[NSA_W:NSA_W + LRU_W]),
            rms_norm(y_cv, g_out[NSA_W + LRU_W:]),
        ], axis=-1)
        x = x + y @ w_out[l]

        hm = rms_norm(x, mlp_norm[l])
        x = x + jnp.square(jax.nn.relu(hm @ mlp_w1[l])) @ mlp_w2[l]
    return x
```

```python
import numpy as np
import ml_dtypes
from contextlib import ExitStack

import concourse.bass as bass
import concourse.mybir as mybir
from concourse.bass_utils import run_bass_kernel_spmd

F32 = mybir.dt.float32
BF16 = mybir.dt.bfloat16
AF = mybir.ActivationFunctionType
ALU = mybir.AluOpType
AX = mybir.AxisListType
NPBF = ml_dtypes.bfloat16

NCORES = 8
D = 1024
S = 16384
NB = 2
DEPTH = 2
N_IN = 2328
EPS = 1e-6
SAME_ENGINE_SYNC = True


class Buf:
    __slots__ = ("name", "w", "r", "dsem", "dcnt")

    def __init__(self, name):
        self.name = name
        self.w = None
        self.r = []
        self.dsem = None
        self.dcnt = 0


class Prog:
    ENG = ("pe", "act", "dve", "pool", "sp")

    def __init__(self, nc, stack):
        self.nc = nc
        self.stack = stack
        self.sem = {k: stack.enter_context(nc.semaphore("sem_" + k)) for k in ("pe", "act", "dve", "pool")}
        self.cnt = {k: 0 for k in self.sem}
        self.lists = {k: [] for k in self.ENG}
        self.waited = {k: {} for k in self.ENG}
        self.dsems = {}
        self.out_events = []
        self.nbuf = 0

    def buf(self, name=None):
        self.nbuf += 1
        return Buf(name or "b%d" % self.nbuf)

    def _waits(self, eng, reads, writes):
        deps = {}

        def add(ev):
            if ev is None:
                return
            k, v = ev
            if deps.get(k, 0) < v:
                deps[k] = v

        for b in reads:
            add(b.w)
        for b in writes:
            add(b.w)
            for ev in b.r:
                add(ev)
        out = []
        for k, v in deps.items():
            if k == eng and (eng == "pe" or not SAME_ENGINE_SYNC):
                continue
            if self.waited[eng].get(k, 0) >= v:
                continue
            self.waited[eng][k] = v
            out.append((k, v))
        return out

    def _semof(self, k):
        return self.sem[k] if isinstance(k, str) else self.dsems[k]

    def op(self, eng, fn, reads=(), writes=()):
        waits = [(self._semof(k), v) for k, v in self._waits(eng, reads, writes)]
        self.cnt[eng] += 1
        ev = (eng, self.cnt[eng])
        sem = self.sem[eng]

        def emit(e):
            for s, v in waits:
                e.wait_ge(s, v)
            fn(e).then_inc(sem, 1)

        self.lists[eng].append(emit)
        for b in reads:
            b.r.append(ev)
        for b in writes:
            b.w = ev
            b.r = []

    def dma(self, q, out, in_, reads=(), writes=(), is_output=False, **kw):
        owner = writes[0] if writes else reads[0]
        if owner.dsem is None:
            owner.dsem = ("d", len(self.dsems))
            self.dsems[owner.dsem] = self.stack.enter_context(self.nc.semaphore("dsem%d" % len(self.dsems)))
        waits = [(self._semof(k), v) for k, v in self._waits(q, reads, writes)]
        owner.dcnt += 16
        ev = (owner.dsem, owner.dcnt)
        sem = self.dsems[owner.dsem]

        def emit(e):
            for s, v in waits:
                e.wait_ge(s, v)
            e.dma_start(out=out, in_=in_, **kw).then_inc(sem, 16)

        self.lists[q].append(emit)
        for b in reads:
            b.r.append(ev)
        for b in writes:
            b.w = ev
            b.r = []
        if is_output:
            self.out_events.append(ev)

    def finish(self):
        fin = {}
        for k, v in self.out_events:
            fin[k] = max(fin.get(k, 0), v)
        waits = [(self._semof(k), v) for k, v in fin.items()]

        def emit(e):
            for s, v in waits:
                e.wait_ge(s, v)

        self.lists["sp"].append(emit)
        lists = self.lists
        with self.nc.Block() as block:
            @block.sync
            def _(e):
                for f in lists["sp"]:
                    f(e)

            @block.tensor
            def _(e):
                for f in lists["pe"]:
                    f(e)

            @block.scalar
            def _(e):
                for f in lists["act"]:
                    f(e)

            @block.vector
            def _(e):
                for f in lists["dve"]:
                    f(e)

            @block.gpsimd
            def _(e):
                for f in lists["pool"]:
                    f(e)


class Ctx:
    def __init__(self, name):
        self.nc = bass.Bass("TRN2", target_bir_lowering=False)
        self.stack = ExitStack()
        self.p = Prog(self.nc, self.stack)
        self.n = 0

    def dram(self, name, shape, dt, kind):
        return self.nc.dram_tensor(name, list(shape), dt, kind=kind).ap()

    def sb(self, shape, dt, name=None):
        self.n += 1
        return self.stack.enter_context(self.nc.sbuf_tensor(name or "sb%d" % self.n, list(shape), dt))

    def ps(self, shape, dt, name=None):
        self.n += 1
        return self.stack.enter_context(self.nc.psum_tensor(name or "ps%d" % self.n, list(shape), dt))


def _run(ctx, in_maps, keep=False):
    if not getattr(ctx, "finished", False):
        ctx.p.finish()
        ctx.finished = True
    res = run_bass_kernel_spmd(ctx.nc, in_maps, core_ids=list(range(NCORES)))
    if not keep:
        ctx.stack.close()
    return res.results


class Ring:
    def __init__(self, ctx, n, shape, dt, name, psum=False):
        self.t = [(ctx.ps if psum else ctx.sb)(shape, dt, "%s%d" % (name, i)) for i in range(n)]
        self.b = [ctx.p.buf("%s%d" % (name, i)) for i in range(n)]
        self.i = -1
        self.n = n

    def next(self):
        self.i = (self.i + 1) % self.n
        return self.t[self.i], self.b[self.i]


def load_weight_scaled(ctx, w_dram, K, N, gcol_t, gcol_b, wb, wb_b, stage, q="sp", col0=0):
    p = ctx.p
    for kc in range(K // 128):
        st, stb = stage.next()
        p.dma(q, st[:, 0:N], w_dram[kc * 128:(kc + 1) * 128, col0:col0 + N], writes=[stb])
        if gcol_t is None:
            p.op("dve", lambda e, st=st, kc=kc: e.tensor_copy(out=wb[:, kc, 0:N], in_=st[:, 0:N]),
                 reads=[stb], writes=[wb_b])
        else:
            p.op("dve", lambda e, st=st, kc=kc: e.tensor_scalar(
                out=wb[:, kc, 0:N], in0=st[:, 0:N], scalar1=gcol_t[:, kc:kc + 1], scalar2=None, op0=ALU.mult),
                reads=[stb, gcol_b], writes=[wb_b])


def rms_rstd(ctx, xt, xb, width, junk, junk_b, small, small_b):
    p = ctx.p
    p.op("act", lambda e: e.activation(out=junk[:, 0:width], in_=xt, func=AF.Square, accum_out=small[:, 0:1]),
         reads=[xb], writes=[junk_b, small_b])
    p.op("dve", lambda e: e.tensor_scalar(out=small[:, 1:2], in0=small[:, 0:1], scalar1=1.0 / width, scalar2=EPS,
                                          op0=ALU.mult, op1=ALU.add), reads=[small_b], writes=[small_b])
    p.op("act", lambda e: e.activation(out=small[:, 2:3], in_=small[:, 1:2], func=AF.Sqrt),
         reads=[small_b], writes=[small_b])
    p.op("dve", lambda e: e.reciprocal(out=small[:, 3:4], in_=small[:, 2:3]), reads=[small_b], writes=[small_b])


TA = 4096
A_CHUNKS = (
    [(c * 128, 128, "nq", c * 128) for c in range(4)]
    + [(512, 128, "raw", 512), (640, 128, "raw", 640), (768, 128, "nk1", 768), (896, 128, "raw", 896),
       (1024, 128, "nk2", 1024), (1152, 128, "raw", 1152)]
    + [(1280, 24, "gate", 0)]
    + [(1304, 128, "rawf", 0), (1432, 128, "rawf", 128), (1560, 128, "gelu", 256), (1688, 128, "gelu", 384)]
    + [(2072, 128, "glu_g", 0), (2200, 128, "glu_g", 1), (1816, 128, "glu_a", 0), (1944, 128, "glu_a", 1)]
)


def build_phase_a(ngroups=TA // 512):
    ctx = Ctx("phA")
    p = ctx.p
    T = ngroups * 512
    x = ctx.dram("x", [T, D], F32, "ExternalInput")
    w = ctx.dram("w", [D, N_IN], F32, "ExternalInput")
    gcol_d = ctx.dram("gcol", [128, 8], F32, "ExternalInput")
    gq_d = ctx.dram("gq", [128, 3], F32, "ExternalInput")
    bo_d = ctx.dram("blockones", [128, 128], F32, "ExternalInput")
    id_d = ctx.dram("ident", [128, 128], BF16, "ExternalInput")
    fa = ctx.dram("fa", [1280, T], BF16, "ExternalOutput")
    gat = ctx.dram("gat", [24, T], F32, "ExternalOutput")
    fl = ctx.dram("fl", [768, T], F32, "ExternalOutput")

    gcol, gcol_b = ctx.sb([128, 8], F32), p.buf()
    gq, gq_b = ctx.sb([128, 3], F32), p.buf()
    bo, bo_b = ctx.sb([128, 128], F32), p.buf()
    ident, id_b = ctx.sb([128, 128], BF16), p.buf()
    p.dma("sp", gcol[:, :], gcol_d[:, :], writes=[gcol_b])
    p.dma("sp", gq[:, :], gq_d[:, :], writes=[gq_b])
    p.dma("sp", bo[:, :], bo_d[:, :], writes=[bo_b])
    p.dma("sp", ident[:, :], id_d[:, :], writes=[id_b])

    wb, wb_b = ctx.sb([128, 8, N_IN], BF16), p.buf()
    stage = Ring(ctx, 2, [128, N_IN], F32, "wst")
    load_weight_scaled(ctx, w, D, N_IN, gcol, gcol_b, wb, wb_b, stage)

    xr = Ring(ctx, 2, [128, D], F32, "xt")
    junk, junk_b = ctx.sb([128, D], BF16), p.buf()
    small = Ring(ctx, 2, [128, 4], F32, "small")
    xn = Ring(ctx, 2, [128, D], BF16, "xn")
    pT = Ring(ctx, 1, [128, 8, 128], BF16, "pT", psum=True)
    hn = Ring(ctx, 2, [128, 8, 512], BF16, "hnT")
    pz = Ring(ctx, 3, [128, 512], F32, "pz", psum=True)
    pss = Ring(ctx, 2, [128, 512], F32, "pss", psum=True)
    sqv = Ring(ctx, 2, [128, 512], F32, "sqv")
    srr = Ring(ctx, 2, [128, 512], F32, "srr")
    ost = Ring(ctx, 3, [128, 512], BF16, "ost")
    osf = Ring(ctx, 3, [128, 512], F32, "osf")
    sg_t = [ctx.sb([128, 512], F32) for _ in range(2)]
    sg_b = [p.buf() for _ in range(2)]

    for g in range(ngroups):
        hT, hb = hn.next()
        for tt in range(4):
            r0 = g * 512 + tt * 128
            xt, xb = xr.next()
            p.dma("sp", xt[:, :], x[r0:r0 + 128, :], writes=[xb])
            sm, smb = small.next()
            rms_rstd(ctx, xt[:, :], xb, D, junk, junk_b, sm, smb)
            xnt, xnb = xn.next()
            p.op("dve", lambda e, xnt=xnt, xt=xt, sm=sm: e.tensor_scalar(
                out=xnt[:, :], in0=xt[:, :], scalar1=sm[:, 3:4], scalar2=None, op0=ALU.mult),
                reads=[xb, smb], writes=[xnb])
            pt, ptb = pT.next()
            for kc in range(8):
                p.op("pe", lambda e, pt=pt, xnt=xnt, kc=kc: e.transpose(
                    out=pt[:, kc, :], in_=xnt[:, kc * 128:(kc + 1) * 128], identity=ident[:, :]),
                    reads=[xnb, id_b], writes=[ptb])
            p.op("act", lambda e, hT=hT, pt=pt, tt=tt: e.copy(out=hT[:, :, tt * 128:(tt + 1) * 128], in_=pt[:, :, :]),
                 reads=[ptb], writes=[hb])
        c0 = g * 512
        for (col0, M, kind, orow) in A_CHUNKS:
            z, zb = pz.next()
            for kc in range(8):
                p.op("pe", lambda e, z=z, kc=kc, col0=col0, M=M, hT=hT: e.matmul(
                    out=z[0:M, :], lhsT=wb[:, kc, col0:col0 + M], rhs=hT[:, kc, :], start=(kc == 0), stop=(kc == 7)),
                    reads=[wb_b, hb], writes=[zb])
            if kind in ("nq", "nk1", "nk2"):
                gi = {"nq": 0, "nk1": 1, "nk2": 2}[kind]
                sc, bi = (1.0, 64.0 * EPS) if kind == "nq" else (1.0 / 64.0, EPS)
                sq, sqb = sqv.next()
                p.op("act", lambda e, sq=sq, z=z: e.activation(out=sq[:, :], in_=z[:, :], func=AF.Square),
                     reads=[zb], writes=[sqb])
                ss, ssb = pss.next()
                p.op("pe", lambda e, ss=ss, sq=sq: e.matmul(out=ss[:, :], lhsT=bo[:, :], rhs=sq[:, :],
                                                            start=True, stop=True),
                     reads=[bo_b, sqb], writes=[ssb])
                sr, srb = srr.next()
                p.op("act", lambda e, sr=sr, ss=ss, sc=sc, bi=bi: e.activation(
                    out=sr[:, :], in_=ss[:, :], func=AF.Sqrt, scale=sc, bias=bi), reads=[ssb], writes=[srb])
                p.op("dve", lambda e, sr=sr: e.reciprocal(out=sr[:, :], in_=sr[:, :]), reads=[srb], writes=[srb])
                o, ob = ost.next()
                p.op("dve", lambda e, o=o, z=z, sr=sr, gi=gi: e.scalar_tensor_tensor(
                    out=o[:, :], in0=z[:, :], scalar=gq[:, gi:gi + 1], in1=sr[:, :], op0=ALU.mult, op1=ALU.mult),
                    reads=[zb, srb, gq_b], writes=[ob])
                p.dma("pool", fa[orow:orow + 128, c0:c0 + 512], o[:, :], reads=[ob], is_output=True)
            elif kind == "raw":
                o, ob = ost.next()
                p.op("act", lambda e, o=o, z=z: e.copy(out=o[:, :], in_=z[:, :]), reads=[zb], writes=[ob])
                p.dma("pool", fa[orow:orow + 128, c0:c0 + 512], o[:, :], reads=[ob], is_output=True)
            elif kind == "gate":
                o, ob = osf.next()
                p.op("act", lambda e, o=o, z=z: e.activation(out=o[0:24, :], in_=z[0:24, :], func=AF.Sigmoid),
                     reads=[zb], writes=[ob])
                p.dma("pool", gat[0:24, c0:c0 + 512], o[0:24, :], reads=[ob], is_output=True)
            elif kind == "rawf":
                o, ob = osf.next()
                p.op("dve", lambda e, o=o, z=z: e.tensor_copy(out=o[:, :], in_=z[:, :]), reads=[zb], writes=[ob])
                p.dma("pool", fl[orow:orow + 128, c0:c0 + 512], o[:, :], reads=[ob], is_output=True)
            elif kind == "gelu":
                o, ob = osf.next()
                p.op("act", lambda e, o=o, z=z: e.activation(out=o[:, :], in_=z[:, :], func=AF.Gelu_apprx_tanh),
                     reads=[zb], writes=[ob])
                p.dma("pool", fl[orow:orow + 128, c0:c0 + 512], o[:, :], reads=[ob], is_output=True)
            elif kind == "glu_g":
                p.op("act", lambda e, z=z, orow=orow: e.activation(out=sg_t[orow][:, :], in_=z[:, :], func=AF.Sigmoid),
                     reads=[zb], writes=[sg_b[orow]])
            elif kind == "glu_a":
                o, ob = osf.next()
                p.op("dve", lambda e, o=o, z=z, orow=orow: e.tensor_tensor(
                    out=o[:, :], in0=z[:, :], in1=sg_t[orow][:, :], op=ALU.mult),
                    reads=[zb, sg_b[orow]], writes=[ob])
                p.dma("pool", fl[512 + orow * 128:512 + (orow + 1) * 128, c0:c0 + 512], o[:, :], reads=[ob],
                      is_output=True)
    return ctx


def consts_a():
    bo = np.zeros((128, 128), np.float32)
    bo[:64, :64] = 1.0
    bo[64:, 64:] = 1.0
    return {"blockones": bo, "ident": np.eye(128, dtype=np.float32).astype(NPBF)}


def run_phase_a(xs, w, attn_norm, q_norm, k_norm, ngroups=TA // 512):
    ctx = build_phase_a(ngroups)
    gq = np.stack([np.tile(q_norm, 2), np.tile(k_norm[1], 2), np.tile(k_norm[2], 2)], axis=1).astype(np.float32)
    common = dict(w=np.ascontiguousarray(w), gcol=np.ascontiguousarray(attn_norm.reshape(8, 128).T),
                  gq=np.ascontiguousarray(gq), **consts_a())
    in_maps = [dict(x=np.ascontiguousarray(xs[c]), **common) for c in range(NCORES)]
    return _run(ctx, in_maps)


def build_phase_c1(ntiles=TA // 128):
    ctx = Ctx("phC1")
    p = ctx.p
    T = ntiles * 128
    x = ctx.dram("x", [T, D], F32, "ExternalInput")
    ya = ctx.dram("ya", [T, 512], F32, "ExternalInput")
    yl = ctx.dram("yl", [T, 256], F32, "ExternalInput")
    yc = ctx.dram("yc", [T, 256], F32, "ExternalInput")
    ln_d = ctx.dram("lnrep", [128, 512], F32, "ExternalInput")
    w = ctx.dram("w", [D, D], F32, "ExternalInput")
    gcol_d = ctx.dram("gcol", [128, 8], F32, "ExternalInput")
    id_d = ctx.dram("ident", [128, 128], BF16, "ExternalInput")
    xo = ctx.dram("xo", [T, D], F32, "ExternalOutput")

    gcol, gcol_b = ctx.sb([128, 8], F32), p.buf()
    ln, ln_b = ctx.sb([128, 512], F32), p.buf()
    ident, id_b = ctx.sb([128, 128], BF16), p.buf()
    p.dma("sp", gcol[:, :], gcol_d[:, :], writes=[gcol_b])
    p.dma("sp", ln[:, :], ln_d[:, :], writes=[ln_b])
    p.dma("sp", ident[:, :], id_d[:, :], writes=[id_b])
    wb, wb_b = ctx.sb([128, 8, D], BF16), p.buf()
    stage = Ring(ctx, 2, [128, D], F32, "wst")
    load_weight_scaled(ctx, w, D, D, gcol, gcol_b, wb, wb_b, stage)

    xr = Ring(ctx, 2, [128, D], F32, "xt")
    yar = Ring(ctx, 2, [128, 512], F32, "ya")
    ylr = Ring(ctx, 2, [128, 256], F32, "yl")
    ycr = Ring(ctx, 2, [128, 256], F32, "yc")
    junk, junk_b = ctx.sb([128, D], BF16), p.buf()
    st6 = Ring(ctx, 2, [128, 8], F32, "st6")
    sm_a = Ring(ctx, 2, [128, 4], F32, "sma")
    sm_l = Ring(ctx, 2, [128, 4], F32, "sml")
    sm_c = Ring(ctx, 2, [128, 4], F32, "smc")
    ycat = Ring(ctx, 2, [128, D], BF16, "ycat")
    pT = Ring(ctx, 2, [128, 8, 128], BF16, "pT", psum=True)
    yT = Ring(ctx, 2, [128, 8, 128], BF16, "yT")
    po = Ring(ctx, 4, [128, 512], F32, "po", psum=True)
    xo_r = Ring(ctx, 2, [128, D], F32, "xo")

    for t in range(ntiles):
        r0 = t * 128
        xt, xb = xr.next()
        p.dma("sp", xt[:, :], x[r0:r0 + 128, :], writes=[xb])
        a, ab = yar.next()
        p.dma("sp", a[:, :], ya[r0:r0 + 128, :], writes=[ab])
        l, lb = ylr.next()
        p.dma("sp", l[:, :], yl[r0:r0 + 128, :], writes=[lb])
        c, cb = ycr.next()
        p.dma("sp", c[:, :], yc[r0:r0 + 128, :], writes=[cb])
        s6, s6b = st6.next()
        p.op("dve", lambda e, s6=s6, c=c: e.bn_stats(out=s6[:, 0:6], in_=c[:, :]), reads=[cb], writes=[s6b])
        p.op("dve", lambda e, s6=s6: e.bn_aggr(out=s6[:, 6:8], in_=s6[:, 0:6]), reads=[s6b], writes=[s6b])
        p.op("dve", lambda e, s6=s6: e.tensor_scalar(out=s6[:, 0:1], in0=s6[:, 7:8], scalar1=EPS, scalar2=None,
                                                     op0=ALU.add), reads=[s6b], writes=[s6b])
        p.op("act", lambda e, s6=s6: e.activation(out=s6[:, 1:2], in_=s6[:, 0:1], func=AF.Sqrt),
             reads=[s6b], writes=[s6b])
        p.op("dve", lambda e, s6=s6: e.reciprocal(out=s6[:, 2:3], in_=s6[:, 1:2]), reads=[s6b], writes=[s6b])
        p.op("dve", lambda e, s6=s6, c=c: e.tensor_scalar(out=c[:, :], in0=c[:, :], scalar1=s6[:, 6:7],
                                                          scalar2=s6[:, 2:3], op0=ALU.subtract, op1=ALU.mult),
             reads=[cb, s6b], writes=[cb])
        p.op("dve", lambda e, c=c: e.tensor_tensor(out=c[:, :], in0=c[:, :], in1=ln[:, 0:256], op=ALU.mult),
             reads=[cb, ln_b], writes=[cb])
        p.op("dve", lambda e, c=c: e.tensor_tensor(out=c[:, :], in0=c[:, :], in1=ln[:, 256:512], op=ALU.add),
             reads=[cb, ln_b], writes=[cb])
        p.op("act", lambda e, c=c: e.activation(out=c[:, :], in_=c[:, :], func=AF.Silu), reads=[cb], writes=[cb])
        yct, ycb = ycat.next()
        for (src, sb_, ring, wdt, off) in ((a, ab, sm_a, 512, 0), (l, lb, sm_l, 256, 512), (c, cb, sm_c, 256, 768)):
            sm, smb = ring.next()
            rms_rstd(ctx, src[:, :], sb_, wdt, junk, junk_b, sm, smb)
            p.op("dve", lambda e, yct=yct, src=src, sm=sm, off=off, wdt=wdt: e.tensor_scalar(
                out=yct[:, off:off + wdt], in0=src[:, :], scalar1=sm[:, 3:4], scalar2=None, op0=ALU.mult),
                reads=[sb_, smb], writes=[ycb])
        pt, ptb = pT.next()
        for kc in range(8):
            p.op("pe", lambda e, pt=pt, yct=yct, kc=kc: e.transpose(
                out=pt[:, kc, :], in_=yct[:, kc * 128:(kc + 1) * 128], identity=ident[:, :]),
                reads=[ycb, id_b], writes=[ptb])
        yt, ytb = yT.next()
        p.op("act", lambda e, yt=yt, pt=pt: e.copy(out=yt[:, :, :], in_=pt[:, :, :]), reads=[ptb], writes=[ytb])
        o, ob = xo_r.next()
        for half in range(2):
            ps, psb = po.next()
            for kc in range(8):
                p.op("pe", lambda e, ps=ps, yt=yt, kc=kc, half=half: e.matmul(
                    out=ps[:, :], lhsT=yt[:, kc, :], rhs=wb[:, kc, half * 512:(half + 1) * 512],
                    start=(kc == 0), stop=(kc == 7)), reads=[ytb, wb_b], writes=[psb])
            p.op("dve", lambda e, o=o, ps=ps, xt=xt, half=half: e.tensor_tensor(
                out=o[:, half * 512:(half + 1) * 512], in0=ps[:, :], in1=xt[:, half * 512:(half + 1) * 512],
                op=ALU.add), reads=[psb, xb], writes=[ob])
        p.dma("pool", xo[r0:r0 + 128, :], o[:, :], reads=[ob], is_output=True)
    return ctx


def run_phase_c1(xs, yas, yls, ycs, w_out, out_norm, ln_g, ln_b, ntiles=TA // 128):
    ctx = build_phase_c1(ntiles)
    lnrep = np.ascontiguousarray(np.broadcast_to(np.concatenate([ln_g, ln_b])[None, :], (128, 512))).astype(np.float32)
    common = dict(w=np.ascontiguousarray(w_out), gcol=np.ascontiguousarray(out_norm.reshape(8, 128).T),
                  lnrep=lnrep, ident=consts_a()["ident"])
    in_maps = [dict(x=np.ascontiguousarray(xs[c]), ya=np.ascontiguousarray(yas[c]), yl=np.ascontiguousarray(yls[c]),
                    yc=np.ascontiguousarray(ycs[c]), **common) for c in range(NCORES)]
    return [r["xo"] for r in _run(ctx, in_maps)]


def build_phase_c2(ngroups=TA // 256):
    ctx = Ctx("phC2")
    p = ctx.p
    T = ngroups * 256
    DF = 4 * D
    x = ctx.dram("x", [T, D], F32, "ExternalInput")
    w1 = ctx.dram("w1", [D, DF], F32, "ExternalInput")
    w2 = ctx.dram("w2", [DF, D], F32, "ExternalInput")
    gcol_d = ctx.dram("gcol", [128, 8], F32, "ExternalInput")
    id_d = ctx.dram("ident", [128, 128], BF16, "ExternalInput")
    xo = ctx.dram("xo", [T, D], F32, "ExternalOutput")

    gcol, gcol_b = ctx.sb([128, 8], F32), p.buf()
    ident, id_b = ctx.sb([128, 128], BF16), p.buf()
    p.dma("sp", gcol[:, :], gcol_d[:, :], writes=[gcol_b])
    p.dma("sp", ident[:, :], id_d[:, :], writes=[id_b])
    w1b, w1b_b = ctx.sb([128, 8, DF], BF16), p.buf()
    w2b, w2b_b = ctx.sb([128, 32, D], BF16), p.buf()
    stage = Ring(ctx, 2, [128, D], F32, "wst")
    for kc in range(8):
        for cq in range(4):
            st, stb = stage.next()
            p.dma("sp", st[:, :], w1[kc * 128:(kc + 1) * 128, cq * D:(cq + 1) * D], writes=[stb])
            p.op("dve", lambda e, st=st, kc=kc, cq=cq: e.tensor_scalar(
                out=w1b[:, kc, cq * D:(cq + 1) * D], in0=st[:, :], scalar1=gcol[:, kc:kc + 1], scalar2=None,
                op0=ALU.mult), reads=[stb, gcol_b], writes=[w1b_b])
    for f in range(32):
        st, stb = stage.next()
        p.dma("sp", st[:, :], w2[f * 128:(f + 1) * 128, :], writes=[stb])
        p.op("pool", lambda e, st=st, f=f: e.tensor_copy(out=w2b[:, f, :], in_=st[:, :]), reads=[stb], writes=[w2b_b])

    xm = Ring(ctx, 4, [128, D], F32, "xm")
    junk, junk_b = ctx.sb([128, D], BF16), p.buf()
    small = Ring(ctx, 2, [128, 4], F32, "small")
    hm = Ring(ctx, 2, [128, D], BF16, "hm")
    pT = Ring(ctx, 2, [128, 8, 128], BF16, "pT", psum=True)
    hmT = Ring(ctx, 2, [128, 8, 256], BF16, "hmT")
    ph = Ring(ctx, 3, [128, 256], F32, "ph", psum=True)
    rl = Ring(ctx, 3, [128, 256], F32, "rl")
    h1 = Ring(ctx, 1, [128, 32, 256], BF16, "h1T")
    po = Ring(ctx, 3, [128, 512], F32, "po", psum=True)

    for g in range(ngroups):
        hT, hTb = hmT.next()
        tiles = []
        for tt in range(2):
            r0 = g * 256 + tt * 128
            xt, xb = xm.next()
            tiles.append((xt, xb, r0))
            p.dma("sp", xt[:, :], x[r0:r0 + 128, :], writes=[xb])
            sm, smb = small.next()
            rms_rstd(ctx, xt[:, :], xb, D, junk, junk_b, sm, smb)
            h, hb = hm.next()
            p.op("dve", lambda e, h=h, xt=xt, sm=sm: e.tensor_scalar(
                out=h[:, :], in0=xt[:, :], scalar1=sm[:, 3:4], scalar2=None, op0=ALU.mult),
                reads=[xb, smb], writes=[hb])
            pt, ptb = pT.next()
            for kc in range(8):
                p.op("pe", lambda e, pt=pt, h=h, kc=kc: e.transpose(
                    out=pt[:, kc, :], in_=h[:, kc * 128:(kc + 1) * 128], identity=ident[:, :]),
                    reads=[hb, id_b], writes=[ptb])
            p.op("act", lambda e, hT=hT, pt=pt, tt=tt: e.copy(out=hT[:, :, tt * 128:(tt + 1) * 128], in_=pt[:, :, :]),
                 reads=[ptb], writes=[hTb])
        h1t, h1b = h1.next()
        for f in range(32):
            ps, psb = ph.next()
            for kc in range(8):
                p.op("pe", lambda e, ps=ps, kc=kc, f=f, hT=hT: e.matmul(
                    out=ps[:, :], lhsT=w1b[:, kc, f * 128:(f + 1) * 128], rhs=hT[:, kc, :],
                    start=(kc == 0), stop=(kc == 7)), reads=[w1b_b, hTb], writes=[psb])
            r, rb = rl.next()
            p.op("act", lambda e, r=r, ps=ps: e.activation(out=r[:, :], in_=ps[:, :], func=AF.Relu),
                 reads=[psb], writes=[rb])
            p.op("dve", lambda e, r=r, ps=ps, f=f, h1t=h1t: e.tensor_tensor(
                out=h1t[:, f, :], in0=ps[:, :], in1=r[:, :], op=ALU.mult), reads=[psb, rb], writes=[h1b])
        for tt in range(2):
            xt, xb, r0 = tiles[tt]
            for half in range(2):
                ps, psb = po.next()
                for f in range(32):
                    p.op("pe", lambda e, ps=ps, f=f, tt=tt, half=half, h1t=h1t: e.matmul(
                        out=ps[:, :], lhsT=h1t[:, f, tt * 128:(tt + 1) * 128],
                        rhs=w2b[:, f, half * 512:(half + 1) * 512], start=(f == 0), stop=(f == 31)),
                        reads=[h1b, w2b_b], writes=[psb])
                p.op("dve", lambda e, ps=ps, xt=xt, half=half: e.tensor_tensor(
                    out=xt[:, half * 512:(half + 1) * 512], in0=ps[:, :], in1=xt[:, half * 512:(half + 1) * 512],
                    op=ALU.add), reads=[psb, xb], writes=[xb])
            p.dma("pool", xo[r0:r0 + 128, :], xt[:, :], reads=[xb], is_output=True)
    return ctx


def run_phase_c2(xs, w1, w2, mlp_norm, ngroups=TA // 256):
    ctx = build_phase_c2(ngroups)
    common = dict(w1=np.ascontiguousarray(w1), w2=np.ascontiguousarray(w2),
                  gcol=np.ascontiguousarray(mlp_norm.reshape(8, 128).T), ident=consts_a()["ident"])
    in_maps = [dict(x=np.ascontiguousarray(xs[c]), **common) for c in range(NCORES)]
    return [r["xo"] for r in _run(ctx, in_maps)]


SEQ_CH = 2048


def build_phase_bseq(nchunks=S // SEQ_CH):
    ctx = Ctx("phBseq")
    p = ctx.p
    T = nchunks * SEQ_CH
    C = 64
    lx = ctx.dram("lx", [C, T], F32, "ExternalInput")
    lg = ctx.dram("lg", [C, T], F32, "ExternalInput")
    cv = ctx.dram("cv", [C, T], F32, "ExternalInput")
    par_d = ctx.dram("par", [C, 40], F32, "ExternalInput")
    bd_d = ctx.dram("bd", [C, 2, C], F32, "ExternalInput")
    id_d = ctx.dram("ident64", [C, C], F32, "ExternalInput")
    yl = ctx.dram("ylT", [C, T], F32, "ExternalOutput")
    yc = ctx.dram("ycT", [C, T], F32, "ExternalOutput")

    par, par_b = ctx.sb([C, 40], F32), p.buf()
    bdf, bdf_b = ctx.sb([C, 2, C], F32), p.buf()
    idf, idf_b = ctx.sb([C, C], F32), p.buf()
    p.dma("sp", par[:, :], par_d[:, :], writes=[par_b])
    p.dma("sp", bdf[:, :, :], bd_d[:, :, :], writes=[bdf_b])
    p.dma("sp", idf[:, :], id_d[:, :], writes=[idf_b])
    bd, bd_b = ctx.sb([C, 2, C], BF16), p.buf()
    p.op("dve", lambda e: e.tensor_copy(out=bd[:, :, :], in_=bdf[:, :, :]), reads=[bdf_b], writes=[bd_b])
    dg, dg_b = ctx.sb([C, 31, C], BF16), p.buf()
    for k in range(31):
        p.op("dve", lambda e, k=k: e.tensor_scalar(out=dg[:, k, :], in0=idf[:, :], scalar1=par[:, 9 + k:10 + k],
                                                   scalar2=None, op0=ALU.mult), reads=[idf_b, par_b], writes=[dg_b])
    cc, cc_b = ctx.sb([C, 4], F32), p.buf()
    p.op("act", lambda e: e.activation(out=cc[:, 0:1], in_=par[:, 7:8], func=AF.Exp, scale=-1.0),
         reads=[par_b], writes=[cc_b])
    p.op("act", lambda e: e.activation(out=cc[:, 1:2], in_=cc[:, 0:1], func=AF.Ln, bias=1.0),
         reads=[cc_b], writes=[cc_b])
    p.op("dve", lambda e: e.tensor_scalar(out=cc[:, 2:3], in0=cc[:, 1:2], scalar1=-8.0, scalar2=None, op0=ALU.mult),
         reads=[cc_b], writes=[cc_b])

    W = SEQ_CH
    lxh = Ring(ctx, 2, [C, 3 + W], F32, "lxh")
    lgr = Ring(ctx, 2, [C, W], F32, "lg")
    cvf = Ring(ctx, 2, [C, 30 + W], F32, "cvf")
    cvb = Ring(ctx, 2, [C, 30 + W], BF16, "cvb")
    xr_r = Ring(ctx, 2, [C, W], F32, "xr")
    xrb_r = Ring(ctx, 2, [C, W], BF16, "xrb")
    r_r = Ring(ctx, 2, [C, W], F32, "r")
    ig_r = Ring(ctx, 2, [C, W], F32, "ig")
    a_r = Ring(ctx, 2, [C, W], F32, "a")
    m_r = Ring(ctx, 2, [C, W], F32, "m")
    h_r = Ring(ctx, 2, [C, W], F32, "h")
    yo_r = Ring(ctx, 2, [C, W], F32, "yo")
    co_r = Ring(ctx, 2, [C, W], F32, "co")
    pg = Ring(ctx, 4, [C, 512], F32, "pg", psum=True)
    pc = Ring(ctx, 3, [C, 512], F32, "pc", psum=True)
    hprev = None

    for ch in range(nchunks):
        c0 = ch * W
        xh, xhb = lxh.next()
        if ch == 0:
            p.op("pool", lambda e, xh=xh: e.memset(xh[:, 0:3], 0.0), writes=[xhb])
            p.dma("sp", xh[:, 3:3 + W], lx[:, 0:W], writes=[xhb])
        else:
            p.dma("sp", xh[:, :], lx[:, c0 - 3:c0 + W], writes=[xhb])
        g, gb = lgr.next()
        p.dma("sp", g[:, :], lg[:, c0:c0 + W], writes=[gb])
        xr, xrb_ = xr_r.next()
        p.op("dve", lambda e, xr=xr, xh=xh: e.tensor_scalar(out=xr[:, :], in0=xh[:, 3:3 + W], scalar1=par[:, 3:4],
                                                            scalar2=par[:, 4:5], op0=ALU.mult, op1=ALU.add),
             reads=[xhb, par_b], writes=[xrb_])
        for k in range(3):
            p.op("dve", lambda e, xr=xr, xh=xh, k=k: e.scalar_tensor_tensor(
                out=xr[:, :], in0=xh[:, k:k + W], scalar=par[:, k:k + 1], in1=xr[:, :], op0=ALU.mult, op1=ALU.add),
                reads=[xhb, par_b, xrb_], writes=[xrb_])
        xb16, xb16b = xrb_r.next()
        p.op("act", lambda e, xb16=xb16, xr=xr: e.copy(out=xb16[:, :], in_=xr[:, :]), reads=[xrb_], writes=[xb16b])
        r, rb = r_r.next()
        ig, igb = ig_r.next()
        for (dst, dstb, wi, bcol) in ((r, rb, 0, 5), (ig, igb, 1, 6)):
            for ct in range(W // 512):
                ps, psb = pg.next()
                p.op("pe", lambda e, ps=ps, wi=wi, ct=ct, xb16=xb16: e.matmul(
                    out=ps[:, :], lhsT=bd[:, wi, :], rhs=xb16[:, ct * 512:(ct + 1) * 512], start=True, stop=True),
                    reads=[bd_b, xb16b], writes=[psb])
                p.op("act", lambda e, ps=ps, dst=dst, ct=ct, bcol=bcol: e.activation(
                    out=dst[:, ct * 512:(ct + 1) * 512], in_=ps[:, :], func=AF.Sigmoid, bias=par[:, bcol:bcol + 1]),
                    reads=[psb, par_b], writes=[dstb])
        a, ab = a_r.next()
        p.op("act", lambda e, a=a, r=r: e.activation(out=a[:, :], in_=r[:, :], func=AF.Exp, scale=cc[:, 2:3]),
             reads=[rb, cc_b], writes=[ab])
        m, mb = m_r.next()
        p.op("pool", lambda e, m=m, a=a: e.tensor_tensor(out=m[:, :], in0=a[:, :], in1=a[:, :], op=ALU.mult),
             reads=[ab], writes=[mb])
        p.op("act", lambda e, m=m: e.activation(out=m[:, :], in_=m[:, :], func=AF.Sqrt, scale=-1.0, bias=1.0),
             reads=[mb], writes=[mb])
        p.op("pool", lambda e, ig=ig, xr=xr: e.tensor_tensor(out=ig[:, :], in0=ig[:, :], in1=xr[:, :], op=ALU.mult),
             reads=[igb, xrb_], writes=[igb])
        p.op("dve", lambda e, ig=ig, m=m: e.tensor_tensor(out=ig[:, :], in0=ig[:, :], in1=m[:, :], op=ALU.mult),
             reads=[igb, mb], writes=[igb])
        h, hb = h_r.next()
        if hprev is None:
            p.op("dve", lambda e, h=h, a=a, ig=ig: e.tensor_tensor_scan(
                out=h[:, :], data0=a[:, :], data1=ig[:, :], initial=0.0, op0=ALU.mult, op1=ALU.add),
                reads=[ab, igb], writes=[hb])
        else:
            hp, hpb = hprev
            p.op("dve", lambda e, h=h, a=a, ig=ig, hp=hp: e.tensor_tensor_scan(
                out=h[:, :], data0=a[:, :], data1=ig[:, :], initial=hp[:, W - 1:W], op0=ALU.mult, op1=ALU.add),
                reads=[ab, igb, hpb], writes=[hb])
        hprev = (h, hb)
        yo, yob = yo_r.next()
        p.op("pool", lambda e, yo=yo, h=h, g=g: e.tensor_tensor(out=yo[:, :], in0=h[:, :], in1=g[:, :], op=ALU.mult),
             reads=[hb, gb], writes=[yob])
        p.dma("pool", yl[:, c0:c0 + W], yo[:, :], reads=[yob], is_output=True)
        cf, cfb = cvf.next()
        if ch == 0:
            p.op("pool", lambda e, cf=cf: e.memset(cf[:, 0:30], 0.0), writes=[cfb])
            p.dma("sp", cf[:, 30:30 + W], cv[:, 0:W], writes=[cfb])
        else:
            p.dma("sp", cf[:, :], cv[:, c0 - 30:c0 + W], writes=[cfb])
        cb16, cb16b = cvb.next()
        p.op("act", lambda e, cb16=cb16, cf=cf: e.copy(out=cb16[:, :], in_=cf[:, :]), reads=[cfb], writes=[cb16b])
        co, cob = co_r.next()
        for ct in range(W // 512):
            ps, psb = pc.next()
            for k in range(31):
                p.op("pe", lambda e, ps=ps, k=k, ct=ct, cb16=cb16: e.matmul(
                    out=ps[:, :], lhsT=dg[:, k, :], rhs=cb16[:, ct * 512 + k:ct * 512 + k + 512],
                    start=(k == 0), stop=(k == 30)), reads=[dg_b, cb16b], writes=[psb])
            p.op("dve", lambda e, ps=ps, co=co, ct=ct: e.tensor_scalar(
                out=co[:, ct * 512:(ct + 1) * 512], in0=ps[:, :], scalar1=par[:, 8:9], scalar2=None, op0=ALU.add),
                reads=[psb, par_b], writes=[cob])
        p.dma("pool", yc[:, c0:c0 + W], co[:, :], reads=[cob], is_output=True)
    return ctx


def bseq_params(inp, l, cc):
    sl = slice(64 * cc, 64 * cc + 64)
    par = np.zeros((64, 40), np.float32)
    par[:, 0:4] = inp["lru_conv_w"][l][:, sl].T
    par[:, 4] = inp["lru_conv_b"][l][sl]
    par[:, 5] = inp["lru_ba"][l][sl]
    par[:, 6] = inp["lru_bx"][l][sl]
    par[:, 7] = inp["lru_lambda"][l][sl]
    par[:, 8] = inp["cv_dw_b"][l][sl]
    par[:, 9:40] = inp["cv_dw_w"][l][:, sl].T
    bd = np.zeros((64, 2, 64), np.float32)
    for hh in range(2):
        bd[32 * hh:32 * hh + 32, 0, 32 * hh:32 * hh + 32] = inp["lru_wa"][l][2 * cc + hh]
        bd[32 * hh:32 * hh + 32, 1, 32 * hh:32 * hh + 32] = inp["lru_wx"][l][2 * cc + hh]
    return dict(par=par, bd=bd, ident64=np.eye(64, dtype=np.float32))


def run_phase_bseq(inp, l, lxs, lgs, cvs, nchunks=S // SEQ_CH):
    ctx = build_phase_bseq(nchunks)
    in_maps = [dict(lx=np.ascontiguousarray(lxs[c]), lg=np.ascontiguousarray(lgs[c]), cv=np.ascontiguousarray(cvs[c]),
                    **bseq_params(inp, l, c % 4)) for c in range(NCORES)]
    res = _run(ctx, in_maps)
    return [r["ylT"] for r in res], [r["ycT"] for r in res]


NEG = -1e30
LOOKAHEAD = 1
DBG_SKIP = set()
ATTN_SPLITS = ((0, 32),)


def build_phase_battn(NJ=32, J0=0):
    ctx = Ctx("phBattn")
    p = ctx.p
    qT_d = ctx.dram("qT", [128, NJ, 512], BF16, "ExternalInput")
    kcin_d = ctx.dram("kcin", [128, S], BF16, "ExternalInput")
    vcin_d = ctx.dram("vcin", [128, S], BF16, "ExternalInput")
    ksel_d = ctx.dram("kselT", [128, S], BF16, "ExternalInput")
    vsel_d = ctx.dram("vsel", [128, 128, 2, 65], BF16, "ExternalInput")
    kw_d = ctx.dram("kw", [128, NJ, 640], BF16, "ExternalInput")
    vw_d = ctx.dram("vw", [128, NJ, 5, 2, 65], BF16, "ExternalInput")
    gat_d = ctx.dram("gates", [128, NJ, 24], F32, "ExternalInput")
    w1_d = ctx.dram("w1rep", [2, 128, 32 * 128], F32, "ExternalInput")
    posT_d = ctx.dram("posT", [64, 2, 32], F32, "ExternalInput")
    w2h_d = ctx.dram("w2h", [128, 256], F32, "ExternalInput")
    w2v_d = ctx.dram("w2v", [128, 64], F32, "ExternalInput")
    kn_d = ctx.dram("kn0", [128, 1], F32, "ExternalInput")
    bo_d = ctx.dram("blockones", [128, 128], F32, "ExternalInput")
    id_d = ctx.dram("ident", [128, 128], BF16, "ExternalInput")
    thr_d = ctx.dram("thr", [128, 32], F32, "ExternalInput")
    iotc_d = ctx.dram("iotaC", [128, 1024], F32, "ExternalInput")
    ft_d = ctx.dram("FT", [128, 512], F32, "ExternalInput")
    cap_d = ctx.dram("CAPT", [128, 512], F32, "ExternalInput")
    c4_d = ctx.dram("causal4", [128, 4, 128], F32, "ExternalInput")
    wm_d = ctx.dram("wmask", [128, 2, 128], F32, "ExternalInput")
    eb_d = ctx.dram("Ebig", [128, 8192], BF16, "ExternalInput")
    yo_d = ctx.dram("yatt", [NJ, 128, 512], F32, "ExternalOutput")

    def const(shape, dt, src):
        t, b = ctx.sb(shape, dt), p.buf()
        full = tuple(slice(None) for _ in shape)
        p.dma("sp", t[full], src[full], writes=[b])
        return t, b

    big, big_b = ctx.sb([128, S], BF16), p.buf()
    p.dma("sp", big[:, :], kcin_d[:, :], writes=[big_b])
    kn, kn_b = const([128, 1], F32, kn_d)
    bo, bo_b = const([128, 128], F32, bo_d)
    ident, id_b = const([128, 128], BF16, id_d)
    thr, thr_b = const([128, 32], F32, thr_d)
    iotc, iotc_b = const([128, 1024], F32, iotc_d)
    FT, FT_b = const([128, 512], F32, ft_d)
    CAPT, CAP_b = const([128, 512], F32, cap_d)
    c4, c4_b = const([128, 4, 128], F32, c4_d)
    wm, wm_b = const([128, 2, 128], F32, wm_d)
    Ebig, eb_b = const([128, 8192], BF16, eb_d)
    gat, gat_b = const([128, NJ, 24], F32, gat_d)
    vsel, vsel_b = const([128, 128, 2, 65], BF16, vsel_d)
    posf, posf_b = const([64, 2, 32], F32, posT_d)
    w2hf, w2hf_b = const([128, 256], F32, w2h_d)
    w2vf, w2vf_b = const([128, 64], F32, w2v_d)
    posb, posb_b = ctx.sb([64, 2, 32], BF16), p.buf()
    w2h, w2h_b = ctx.sb([128, 2, 128], BF16), p.buf()
    w2v, w2v_b = ctx.sb([128, 64], BF16), p.buf()
    p.op("dve", lambda e: e.tensor_copy(out=posb[:, :, :], in_=posf[:, :, :]), reads=[posf_b], writes=[posb_b])
    p.op("dve", lambda e: e.tensor_copy(out=w2h[:, :, :], in_=w2hf[:, :].rearrange("p (h m) -> p h m", h=2)),
         reads=[w2hf_b], writes=[w2h_b])
    p.op("dve", lambda e: e.tensor_copy(out=w2v[:, :], in_=w2vf[:, :]), reads=[w2vf_b], writes=[w2v_b])

    pS = Ring(ctx, 3, [128, 512], F32, "pS", psum=True)
    pmx_r = Ring(ctx, 2, [128, 512], F32, "pmx", psum=True)
    acc = Ring(ctx, 2, [128, 4, 128], F32, "acc", psum=True)
    pT_t = ctx.ps([128, 8, 128], BF16, "pT")
    pT_b = [p.buf() for _ in range(8)]
    pTi = [0]
    pmi = [0]

    def next_pT():
        pTi[0] = (pTi[0] + 1) % 8
        return pT_t[:, pTi[0], :], pT_b[pTi[0]]

    def next_pmx():
        t, b = pmx_r.next()
        return t[:, 0:128], b

    kcT, kcT_b = ctx.sb([128, 1024], BF16), p.buf()
    vc, vc_b = ctx.sb([128, 8, 2, 65], BF16), p.buf()
    p.op("pool", lambda e: e.memset(kcT[:, :], 0.0), writes=[kcT_b])
    p.op("pool", lambda e: e.memset(vc[:, :, :, :], 1.0), writes=[vc_b])
    w1st, w1st_b = ctx.sb([128, 4096], F32), p.buf()
    w1r, w1r_b = ctx.sb([128, 32, 128], BF16), p.buf()
    hid = [ctx.sb([128, 1024], BF16) for _ in range(2)]
    hid_b = [p.buf() for _ in range(2)]
    b1, b1_b = ctx.sb([128, 2], F32), p.buf()
    sqv = Ring(ctx, 2, [128, 512], F32, "sqv")
    bigv = big[:, :].rearrange("p (n s) -> p n s", s=16)
    for kv in range(2):
        if kv == 1:
            p.dma("sp", big[:, :], vcin_d[:, :], writes=[big_b])
        p.dma("sp", w1st[:, :], w1_d[kv, :, :], writes=[w1st_b])
        p.op("dve", lambda e: e.tensor_copy(out=w1r[:, :, :], in_=w1st[:, :].rearrange("p (a b) -> p a b", a=32)),
             reads=[w1st_b], writes=[w1r_b])
        ps, psb = pS.next()
        for pp in range(32):
            p.op("pe", lambda e, ps=ps, pp=pp, kv=kv: e.matmul(
                out=ps[:, 0:1], lhsT=w1r[0:64, pp, :], rhs=posb[0:64, kv, pp:pp + 1], start=(pp == 0), stop=(pp == 31)),
                reads=[w1r_b, posb_b], writes=[psb])
        p.op("dve", lambda e, ps=ps, kv=kv: e.tensor_copy(out=b1[:, kv:kv + 1], in_=ps[:, 0:1]),
             reads=[psb], writes=[b1_b])
        for h in range(2):
            p.op("pool", lambda e, h=h: e.memset(hid[h][:, :], 0.0), writes=[hid_b[h]])
            for nt in range(2):
                N = 512 if nt == 0 else 511
                ps, psb = pS.next()
                for pp in range(32):
                    n0 = nt * 512 + pp // 16
                    p.op("pe", lambda e, ps=ps, pp=pp, h=h, n0=n0, N=N: e.matmul(
                        out=ps[:, 0:N], lhsT=w1r[h * 64:(h + 1) * 64, pp, :],
                        rhs=bigv[h * 64:(h + 1) * 64, n0:n0 + N, pp % 16], start=(pp == 0), stop=(pp == 31)),
                        reads=[w1r_b, big_b], writes=[psb])
                p.op("act", lambda e, ps=ps, h=h, nt=nt, N=N, kv=kv: e.activation(
                    out=hid[h][:, nt * 512:nt * 512 + N], in_=ps[:, 0:N], func=AF.Gelu_apprx_tanh,
                    bias=b1[:, kv:kv + 1]), reads=[psb, b1_b], writes=[hid_b[h]])
        if kv == 0:
            for nt in range(2):
                N = 512 if nt == 0 else 511
                ps, psb = pS.next()
                for h in range(2):
                    p.op("pe", lambda e, ps=ps, h=h, nt=nt, N=N: e.matmul(
                        out=ps[:, 0:N], lhsT=w2h[:, h, :], rhs=hid[h][:, nt * 512:nt * 512 + N],
                        start=(h == 0), stop=(h == 1)), reads=[w2h_b, hid_b[h]], writes=[psb])
                sq, sqb = sqv.next()
                p.op("act", lambda e, sq=sq, ps=ps, N=N: e.activation(out=sq[:, 0:N], in_=ps[:, 0:N], func=AF.Square),
                     reads=[psb], writes=[sqb])
                ss, ssb = pS.next()
                p.op("pe", lambda e, ss=ss, sq=sq, N=N: e.matmul(out=ss[:, 0:N], lhsT=bo[:, :], rhs=sq[:, 0:N],
                                                                 start=True, stop=True),
                     reads=[bo_b, sqb], writes=[ssb])
                p.op("act", lambda e, sq=sq, ss=ss, N=N: e.activation(
                    out=sq[:, 0:N], in_=ss[:, 0:N], func=AF.Sqrt, scale=1.0 / 64.0, bias=EPS),
                    reads=[ssb], writes=[sqb])
                p.op("dve", lambda e, sq=sq, N=N: e.reciprocal(out=sq[:, 0:N], in_=sq[:, 0:N]),
                     reads=[sqb], writes=[sqb])
                p.op("dve", lambda e, sq=sq, ps=ps, nt=nt, N=N: e.scalar_tensor_tensor(
                    out=kcT[:, nt * 512:nt * 512 + N], in0=ps[:, 0:N], scalar=kn[:, 0:1], in1=sq[:, 0:N],
                    op0=ALU.mult, op1=ALU.mult), reads=[psb, sqb, kn_b], writes=[kcT_b])
        else:
            for h in range(2):
                for c in range(8):
                    ps, psb = pS.next()
                    p.op("pe", lambda e, ps=ps, h=h, c=c: e.matmul(
                        out=ps[:, 0:64], lhsT=hid[h][:, c * 128:(c + 1) * 128], rhs=w2v[:, :], start=True, stop=True),
                        reads=[hid_b[h], w2v_b], writes=[psb])
                    p.op("act", lambda e, ps=ps, h=h, c=c: e.copy(out=vc[:, c, h, 0:64], in_=ps[:, 0:64]),
                         reads=[psb], writes=[vc_b])
    p.dma("sp", big[:, :], ksel_d[:, :], writes=[big_b])
    ksel, ksel_b = big, big_b

    qr = Ring(ctx, 2, [128, 512], BF16, "q")
    kwr = Ring(ctx, 2, [128, 640], BF16, "kw")
    vwr = Ring(ctx, 2, [128, 5, 2, 65], BF16, "vw")
    ecmp = [ctx.sb([128, 1024], F32) for _ in range(4)]
    ecmp_b = [p.buf() for _ in range(4)]
    cm, cm_b = ctx.sb([128, 1024], F32), p.buf()
    imp, imp_b = ctx.sb([128, 1024], F32), p.buf()
    den, den_b = ctx.sb([128, 8], F32), p.buf()
    pb16 = Ring(ctx, 2, [128, 1024], BF16, "pb16")
    pTs = Ring(ctx, 3, [128, 128], BF16, "pTs")
    sc, sc_b = ctx.sb([128, 256], F32), p.buf()
    sc2, sc2_b = ctx.sb([128, 256], F32), p.buf()
    m8, m8_b = ctx.sb([128, 16], F32), p.buf()
    selm = [ctx.sb([128, 128], BF16) for _ in range(2)]
    selm_b = p.buf()
    for hf_ in range(2):
        p.op("pool", lambda e, hf_=hf_: e.memset(selm[hf_][:, :], 0.0), writes=[selm_b])
    mT = Ring(ctx, 4, [128, 128], BF16, "mT")
    er = Ring(ctx, 5, [128, 512], BF16, "e")
    ptr = Ring(ctx, 5, [128, 4, 128], BF16, "pts")
    mkr = Ring(ctx, 4, [128, 128], F32, "mk")
    fr = Ring(ctx, 2, [128, 8], F32, "f")
    oacc = Ring(ctx, 2, [128, 512], F32, "oacc")

    def evac(ac, acb, j, h, br, o, ob, first):
        f, fb = fr.next()
        p.op("dve", lambda e: e.tensor_scalar(out=f[:, 0:4], in0=ac[:, :, 64], scalar1=1e-30, scalar2=None,
                                              op0=ALU.max), reads=[acb], writes=[fb])
        p.op("dve", lambda e: e.reciprocal(out=f[:, 0:4], in_=f[:, 0:4]), reads=[fb], writes=[fb])
        p.op("dve", lambda e: e.tensor_tensor(out=f[:, 4:8], in0=f[:, 0:4],
                                              in1=gat[:, j, h * 12 + br:h * 12 + 12:3], op=ALU.mult),
             reads=[fb, gat_b], writes=[fb])
        ov = o[:, h * 256:(h + 1) * 256].rearrange("p (g d) -> p g d", g=4)
        fbc = f[:, 4:8].unsqueeze(2).broadcast_to([128, 4, 64])
        if first:
            p.op("dve", lambda e: e.tensor_tensor(out=ov, in0=ac[:, :, 0:64], in1=fbc, op=ALU.mult),
                 reads=[acb, fb], writes=[ob])
        else:
            t, tb = mkr.next()
            for half in range(2):
                tv = t[:, :].rearrange("p (g d) -> p g d", g=2)
                p.op("dve", lambda e, half=half, tv=tv: e.tensor_tensor(
                    out=tv, in0=ac[:, 2 * half:2 * half + 2, 0:64],
                    in1=f[:, 4 + 2 * half:6 + 2 * half].unsqueeze(2).broadcast_to([128, 2, 64]), op=ALU.mult),
                    reads=[acb, fb], writes=[tb])
                p.op("dve", lambda e, half=half, tv=tv: e.tensor_tensor(
                    out=ov[:, 2 * half:2 * half + 2, :], in0=ov[:, 2 * half:2 * half + 2, :], in1=tv, op=ALU.add),
                    reads=[tb, ob], writes=[ob])

    for j in range(J0, NJ):
        nb = 8 * (j + 1)
        Nv = 32 * (j + 1)
        nsel = 4 * j + 4
        q, qb = qr.next()
        p.dma("sp", q[:, :], qT_d[:, j, :], writes=[qb])
        kw, kwb = kwr.next()
        p.dma("sp", kw[:, :], kw_d[:, j, :], writes=[kwb])
        vw, vwb = vwr.next()
        p.dma("sp", vw[:, :, :, :], vw_d[:, j, :, :, :], writes=[vwb])
        p.op("dve", lambda e, j=j, Nv=Nv: e.tensor_scalar(out=cm[:, 0:Nv], in0=iotc[:, 0:Nv], scalar1=thr[:, j:j + 1],
                                                          scalar2=None, op0=ALU.is_le),
             reads=[iotc_b, thr_b], writes=[cm_b])
        o, ob = oacc.next()
        for h in range(2):
            hs = slice(h * 64, (h + 1) * 64)
            for g in range(4):
                for nt in range((Nv + 511) // 512):
                    n0, n1 = nt * 512, min(Nv, nt * 512 + 512)
                    ps, psb = pS.next()
                    p.op("pe", lambda e, ps=ps, g=g, n0=n0, n1=n1, q=q, hs=hs: e.matmul(
                        out=ps[:, 0:n1 - n0], lhsT=q[hs, g * 128:(g + 1) * 128], rhs=kcT[hs, n0:n1],
                        start=True, stop=True), reads=[qb, kcT_b], writes=[psb])
                    p.op("act", lambda e, ps=ps, g=g, n0=n0, n1=n1: e.activation(
                        out=ecmp[g][:, n0:n1], in_=ps[:, 0:n1 - n0], func=AF.Exp), reads=[psb], writes=[ecmp_b[g]])
                p.op("dve", lambda e, g=g, Nv=Nv: e.scalar_tensor_tensor(
                    out=ecmp[g][:, 0:Nv], in0=ecmp[g][:, 0:Nv], scalar=1.0, in1=cm[:, 0:Nv], op0=ALU.mult,
                    op1=ALU.mult, accum_out=den[:, g:g + 1]), reads=[ecmp_b[g], cm_b], writes=[ecmp_b[g], den_b])
            p.op("dve", lambda e: e.tensor_scalar(out=den[:, 4:8], in0=den[:, 0:4], scalar1=1e-30, scalar2=None,
                                                  op0=ALU.max), reads=[den_b], writes=[den_b])
            p.op("dve", lambda e: e.reciprocal(out=den[:, 4:8], in_=den[:, 4:8]), reads=[den_b], writes=[den_b])
            p.op("dve", lambda e, Nv=Nv: e.tensor_scalar(out=imp[:, 0:Nv], in0=ecmp[0][:, 0:Nv], scalar1=den[:, 4:5],
                                                         scalar2=None, op0=ALU.mult),
                 reads=[ecmp_b[0], den_b], writes=[imp_b])
            for g in range(1, 4):
                p.op("dve", lambda e, g=g, Nv=Nv: e.scalar_tensor_tensor(
                    out=imp[:, 0:Nv], in0=ecmp[g][:, 0:Nv], scalar=den[:, 4 + g:5 + g], in1=imp[:, 0:Nv],
                    op0=ALU.mult, op1=ALU.add), reads=[ecmp_b[g], den_b, imp_b], writes=[imp_b])
            ac, acb = acc.next()
            nch = (Nv + 127) // 128
            for g in range(4):
                pb, pbb = pb16.next()
                p.op("act", lambda e, pb=pb, g=g, Nv=Nv: e.copy(out=pb[:, 0:Nv], in_=ecmp[g][:, 0:Nv]),
                     reads=[ecmp_b[g]], writes=[pbb])
                for c in range(nch):
                    w = min(128, Nv - c * 128)
                    pt, ptb = next_pT()
                    p.op("pe", lambda e, pt=pt, pb=pb, c=c, w=w: e.transpose(
                        out=pt[0:w, :], in_=pb[:, c * 128:c * 128 + w], identity=ident[:, :]),
                        reads=[pbb, id_b], writes=[ptb])
                    ts, tsb = pTs.next()
                    p.op("act", lambda e, ts=ts, pt=pt, w=w: e.copy(out=ts[0:w, :], in_=pt[0:w, :]),
                         reads=[ptb], writes=[tsb])
                    p.op("pe", lambda e, ac=ac, ts=ts, g=g, c=c, w=w, h=h, nch=nch: e.matmul(
                        out=ac[:, g, 0:65], lhsT=ts[0:w, :], rhs=vc[0:w, c, h, :],
                        start=(g == 0 and c == 0), stop=(g == 3 and c == nch - 1), skip_group_check=True),
                        reads=[tsb, vc_b], writes=[acb])
            evac(ac, acb, j, h, 0, o, ob, True)
            if 'topk' in DBG_SKIP:
                continue
            p.op("dve", lambda e, nb=nb: e.tensor_reduce(
                out=sc[:, 0:nb], in_=imp[:, 0:4 * nb].rearrange("p (b r) -> p b r", r=4), axis=AX.X, op=ALU.add),
                reads=[imp_b], writes=[sc_b])
            p.op("dve", lambda e, nb=nb: e.tensor_tensor(out=sc[:, 1:nb], in0=sc[:, 1:nb], in1=imp[:, 3:4 * nb - 1:4],
                                                         op=ALU.add), reads=[imp_b, sc_b], writes=[sc_b])
            f0 = 256 - 8 * j
            p.op("dve", lambda e, nb=nb, f0=f0: e.tensor_tensor(out=sc[:, 0:nb], in0=sc[:, 0:nb],
                                                                in1=FT[:, f0:f0 + nb], op=ALU.add),
                 reads=[sc_b, FT_b], writes=[sc_b])
            p.op("dve", lambda e, nb=nb, f0=f0: e.tensor_tensor(out=sc[:, 0:nb], in0=sc[:, 0:nb],
                                                                in1=CAPT[:, f0:f0 + nb], op=ALU.min),
                 reads=[sc_b, CAP_b], writes=[sc_b])
            p.op("dve", lambda e: e.memset(sc[:, 0:1], 1e6), reads=[], writes=[sc_b])
            if nb >= 24 and 'tk_sort' not in DBG_SKIP:
                p.op("dve", lambda e, nb=nb: e.max(out=m8[:, 0:8], in_=sc[:, 0:nb]), reads=[sc_b], writes=[m8_b])
                p.op("dve", lambda e, nb=nb: e.match_replace(out=sc2[:, 0:nb], in_to_replace=m8[:, 0:8],
                                                             in_values=sc[:, 0:nb], imm_value=-2e30),
                     reads=[sc_b, m8_b], writes=[sc2_b])
                p.op("dve", lambda e, nb=nb: e.max(out=m8[:, 8:16], in_=sc2[:, 0:nb]), reads=[sc2_b], writes=[m8_b])
                p.op("dve", lambda e: e.tensor_scalar(out=m8[:, 0:1], in0=m8[:, 15:16], scalar1=-1.0, scalar2=None,
                                                      op0=ALU.max), reads=[m8_b], writes=[m8_b])
                for hf in range((nb + 127) // 128):
                    c0_, c1_ = hf * 128, min(nb, hf * 128 + 128)
                    p.op("dve", lambda e, hf=hf, c0_=c0_, c1_=c1_: e.tensor_scalar(
                        out=selm[hf][:, 0:c1_ - c0_], in0=sc[:, c0_:c1_], scalar1=m8[:, 0:1], scalar2=None,
                        op0=ALU.is_ge), reads=[sc_b, m8_b], writes=[selm_b])
            else:
                p.op("dve", lambda e, nb=nb: e.tensor_scalar(out=selm[0][:, 0:nb], in0=sc[:, 0:nb], scalar1=-1.0,
                                                             scalar2=None, op0=ALU.is_ge),
                     reads=[sc_b], writes=[selm_b])
            mts = []
            for hf in range(0 if 'tk_tr' in DBG_SKIP else (nb + 127) // 128):
                mt, mtb = mT.next()
                mts.append((mt, mtb))
                pt, ptb = next_pT()
                p.op("pe", lambda e, pt=pt, hf=hf: e.transpose(
                    out=pt, in_=selm[hf][:, :], identity=ident[:, :]),
                    reads=[selm_b, id_b], writes=[ptb])
                p.op("act", lambda e, mt=mt, pt=pt: e.copy(out=mt[:, :], in_=pt), reads=[ptb], writes=[mtb])
            if 'sel' in DBG_SKIP:
                continue
            qv = q[hs, :]
            acs, acsb = acc.next()
            acw, acwb = acc.next()
            descs = []
            for kc in range(nsel):
                descs.append(dict(k=ksel[hs, kc * 128:(kc + 1) * 128], kb=ksel_b,
                                  mask=("selc", kc) if kc >= 4 * j else ("sel", kc), v=vsel[:, kc, h, :], vb=vsel_b,
                                  ac=acs, acb=acsb, first=(kc == 0), last=(kc == nsel - 1), br=1))
            if 'win' not in DBG_SKIP:
                for m in range(5):
                    descs.append(dict(k=kw[hs, m * 128:(m + 1) * 128], kb=kwb,
                                      mask=("win", 0) if m == 0 else (("win", 1) if m == 4 else None),
                                      v=vw[:, m, h, :], vb=vwb, ac=acw, acb=acwb, first=(m == 0), last=(m == 4), br=2))

            def stage1(dsc, qv=qv, j=j, mts=mts, qb=qb):
                ps, psb = pS.next()
                p.op("pe", lambda e: e.matmul(out=ps[:, :], lhsT=dsc["k"], rhs=qv, start=True, stop=True),
                     reads=[dsc["kb"], qb], writes=[psb])
                mk_ = dsc["mask"]
                if mk_ is not None and mk_[0] in ("sel", "selc"):
                    kc = mk_[1]
                    mt, mtb = mts[kc // 64]
                    mx, mxb = next_pmx()
                    p.op("pe", lambda e: e.matmul(
                        out=mx, lhsT=Ebig[:, 128 * (kc % 64):128 * (kc % 64) + 128], rhs=mt[:, :],
                        start=True, stop=True), reads=[eb_b, mtb], writes=[mxb])
                et, etb = er.next()
                p.op("act", lambda e: e.activation(out=et[:, :], in_=ps[:, :], func=AF.Exp),
                     reads=[psb], writes=[etb])
                ev = et[:, :].rearrange("p (g q) -> p g q", g=4)
                if mk_ is None:
                    return ev, etb
                pt_, ptb_ = ptr.next()
                if mk_[0] == "selc":
                    mk, mkb = mkr.next()
                    p.op("dve", lambda e: e.tensor_tensor(out=mk[:, :], in0=mx, in1=c4[:, mk_[1] - 4 * j, :],
                                                          op=ALU.mult), reads=[mxb, c4_b], writes=[mkb])
                    p.op("dve", lambda e: e.tensor_tensor(
                        out=pt_[:, :, :], in0=ev, in1=mk[:, :].unsqueeze(1).broadcast_to([128, 4, 128]), op=ALU.mult),
                        reads=[etb, mkb], writes=[ptb_])
                elif mk_[0] == "sel":
                    p.op("dve", lambda e: e.tensor_tensor(
                        out=pt_[:, :, :], in0=ev, in1=mx.unsqueeze(1).broadcast_to([128, 4, 128]), op=ALU.mult),
                        reads=[etb, mxb], writes=[ptb_])
                else:
                    wi = mk_[1]
                    p.op("dve", lambda e: e.tensor_tensor(
                        out=pt_[:, :, :], in0=ev, in1=wm[:, wi, :].unsqueeze(1).broadcast_to([128, 4, 128]),
                        op=ALU.mult), reads=[etb, wm_b], writes=[ptb_])
                return pt_, ptb_

            def stage2(dsc, src, srcb, j=j, h=h, o=o, ob=ob):
                for g in range(4):
                    p.op("pe", lambda e, g=g: e.matmul(
                        out=dsc["ac"][:, g, 0:65], lhsT=src[:, g, :], rhs=dsc["v"],
                        start=(g == 0 and dsc["first"]), stop=(g == 3 and dsc["last"]), skip_group_check=True),
                        reads=[srcb, dsc["vb"]], writes=[dsc["acb"]])
                if dsc["last"]:
                    evac(dsc["ac"], dsc["acb"], j, h, dsc["br"], o, ob, False)

            pend = []
            for i, dsc in enumerate(descs):
                pend.append((dsc,) + stage1(dsc))
                if len(pend) > LOOKAHEAD:
                    stage2(*pend.pop(0))
            while pend:
                stage2(*pend.pop(0))
        p.dma("pool", yo_d[j, :, :], o[:, :], reads=[ob], is_output=True)
    return ctx


def battn_consts(cc):
    qi = np.arange(128)
    n = np.arange(1024)
    thr = np.broadcast_to((128.0 * (4 * np.arange(32) + cc))[None, :], (128, 32)).astype(np.float32)
    iotaC = (16.0 * n[None, :] + 31.0 - qi[:, None]).astype(np.float32)
    rp = np.arange(512) - 256
    hi = (qi >= 64).astype(np.int64)[:, None]
    forced = (rp[None, :] == 2 * cc + hi) | (rp[None, :] == 2 * cc + hi - 1)
    valid = rp[None, :] <= 2 * cc + hi
    FT = np.where(forced, 1e6, 0.0).astype(np.float32)
    CAPT = np.where(valid, 3e6, NEG).astype(np.float32)
    k = np.arange(128)
    c4 = np.stack([(k[:, None] - qi[None, :] <= 128 * (cc - dd)) for dd in range(4)], axis=1).astype(np.float32)
    wm = np.stack([k[:, None] > qi[None, :], k[:, None] <= qi[None, :]], axis=1).astype(np.float32)
    Ebig = (np.arange(8192)[None, :] // 64 == np.arange(128)[:, None]).astype(np.float32).astype(NPBF)
    return dict(thr=np.ascontiguousarray(thr), iotaC=iotaC, FT=FT, CAPT=CAPT, causal4=c4, wmask=wm, Ebig=Ebig,
                **consts_a())


def battn_inputs(inp, l, fa_b, gat_b, cc, NJ=32):
    blocks = 4 * np.arange(NJ) + cc
    q = fa_b[0:512].reshape(2, 4, 64, S // 128, 128)[:, :, :, blocks, :]
    qT = np.ascontiguousarray(q.transpose(0, 2, 3, 1, 4).reshape(128, NJ, 512))
    vs = fa_b[896:1024].reshape(2, 64, 128, 128).transpose(3, 2, 0, 1)
    vsel = np.ones((128, 128, 2, 65), NPBF)
    vsel[..., 0:64] = vs
    kwp = np.concatenate([np.zeros((128, 512), NPBF), fa_b[1024:1152]], axis=1)
    vwp = np.concatenate([np.zeros((128, 512), NPBF), fa_b[1152:1280]], axis=1)
    valid = np.concatenate([np.zeros(512, NPBF), np.ones(S, NPBF)])
    kw = np.stack([kwp[:, 128 * i:128 * i + 640] for i in blocks], axis=1)
    vw = np.zeros((128, NJ, 5, 2, 65), NPBF)
    for jj, i in enumerate(blocks):
        seg = vwp[:, 128 * i:128 * i + 640].reshape(2, 64, 5, 128)
        vw[:, jj, :, :, 0:64] = seg.transpose(3, 2, 0, 1)
        vw[:, jj, :, :, 64] = valid[128 * i:128 * i + 640].reshape(5, 128).T[:, :, None]
    gates = np.ascontiguousarray(gat_b.reshape(24, S // 128, 128)[:, blocks, :].transpose(2, 1, 0))
    w1 = inp["cmp_w1"][l]
    w1rep = np.stack([np.tile(w1[kv].reshape(32, 64, 128).transpose(1, 0, 2).reshape(64, 4096), (2, 1))
                      for kv in range(2)], axis=0)
    posT = np.ascontiguousarray(inp["cmp_pos"][l].transpose(2, 0, 1))
    w2 = inp["cmp_w2"][l]
    w2h = np.zeros((128, 2, 128), np.float32)
    w2h[:, 0, 0:64] = w2[0]
    w2h[:, 1, 64:128] = w2[0]
    return dict(qT=qT, kcin=np.ascontiguousarray(fa_b[512:640]), vcin=np.ascontiguousarray(fa_b[640:768]),
                kselT=np.ascontiguousarray(fa_b[768:896]), vsel=vsel, kw=np.ascontiguousarray(kw), vw=vw,
                gates=gates.astype(np.float32), w1rep=np.ascontiguousarray(w1rep.astype(np.float32)), posT=posT,
                w2h=w2h.reshape(128, 256), w2v=np.ascontiguousarray(w2[1]),
                kn0=np.tile(inp["k_norm"][l][0], 2).reshape(128, 1).astype(np.float32), **battn_consts(cc))


def run_phase_battn(inp, l, fa_seq, gat_seq, NJ=32):
    ctx = build_phase_battn(NJ)
    in_maps = [battn_inputs(inp, l, fa_seq[c // 4], gat_seq[c // 4], c % 4, NJ) for c in range(NCORES)]
    res = _run(ctx, in_maps)
    y = np.zeros((NB, S // 128, 128, 512), np.float32)
    for c in range(NCORES):
        y[c // 4, 4 * np.arange(NJ) + c % 4] = np.asarray(res[c]["yatt"])
    return y.reshape(NB, S, 512)


def kernel(**inputs):
    inp = {k: np.asarray(v) for k, v in inputs.items()}
    x = np.ascontiguousarray(inp["x"], dtype=np.float32).reshape(NB * S, D)
    xs = [x[c * TA:(c + 1) * TA] for c in range(NCORES)]
    cBa = ATTN_SPLITS
    ca = consts_a()
    for l in range(DEPTH):
        last = l == DEPTH - 1
        gq = np.stack([np.tile(inp["q_norm"][l], 2), np.tile(inp["k_norm"][l][1], 2),
                       np.tile(inp["k_norm"][l][2], 2)], axis=1).astype(np.float32)
        common = dict(w=np.ascontiguousarray(inp["w_in"][l]),
                      gcol=np.ascontiguousarray(inp["attn_norm"][l].reshape(8, 128).T), gq=np.ascontiguousarray(gq), **ca)
        ra = _run(build_phase_a(), [dict(x=np.ascontiguousarray(xs[c]), **common) for c in range(NCORES)])
        fa_seq = [np.concatenate([np.asarray(ra[4 * b + q]["fa"]) for q in range(4)], axis=1) for b in range(NB)]
        if fa_seq[0].dtype != NPBF:
            fa_seq = [a.astype(NPBF) for a in fa_seq]
        gat_seq = [np.concatenate([np.asarray(ra[4 * b + q]["gat"]) for q in range(4)], axis=1) for b in range(NB)]
        fl_seq = [np.concatenate([np.asarray(ra[4 * b + q]["fl"]) for q in range(4)], axis=1) for b in range(NB)]
        del ra
        y_attn = np.zeros((NB, S // 128, 128, 512), np.float32)
        full = [battn_inputs(inp, l, fa_seq[c // 4], gat_seq[c // 4], c % 4) for c in range(NCORES)]
        for (j0, j1) in cBa:
            maps = []
            for c in range(NCORES):
                m = dict(full[c])
                for k in ("qT", "kw", "vw", "gates"):
                    m[k] = np.ascontiguousarray(m[k][:, 0:j1])
                maps.append(m)
            rb = _run(build_phase_battn(j1, j0), maps)
            for c in range(NCORES):
                y_attn[c // 4, 4 * np.arange(j0, j1) + c % 4] = np.asarray(rb[c]["yatt"])[j0:j1]
            del rb, maps
        y_attn = y_attn.reshape(NB * S, 512)
        del full, fa_seq, gat_seq
        maps = []
        for c in range(NCORES):
            b, cc = c // 4, c % 4
            sl = slice(64 * cc, 64 * cc + 64)
            maps.append(dict(lx=np.ascontiguousarray(fl_seq[b][0:256][sl]), lg=np.ascontiguousarray(fl_seq[b][256:512][sl]),
                             cv=np.ascontiguousarray(fl_seq[b][512:768][sl]), **bseq_params(inp, l, cc)))
        rs = _run(build_phase_bseq(), maps)
        y_lru = np.concatenate([np.concatenate([np.asarray(rs[4 * b + q]["ylT"]) for q in range(4)], axis=0).T
                                for b in range(NB)], axis=0)
        y_cv = np.concatenate([np.concatenate([np.asarray(rs[4 * b + q]["ycT"]) for q in range(4)], axis=0).T
                               for b in range(NB)], axis=0)
        del rs, fl_seq, maps
        lnrep = np.ascontiguousarray(np.broadcast_to(
            np.concatenate([inp["cv_ln_g"][l], inp["cv_ln_b"][l]])[None, :], (128, 512))).astype(np.float32)
        common = dict(w=np.ascontiguousarray(inp["w_out"][l]),
                      gcol=np.ascontiguousarray(inp["out_norm"][l].reshape(8, 128).T), lnrep=lnrep, ident=ca["ident"])
        r1 = _run(build_phase_c1(), [dict(x=np.ascontiguousarray(xs[c]), ya=np.ascontiguousarray(y_attn[c * TA:(c + 1) * TA]),
                             yl=np.ascontiguousarray(y_lru[c * TA:(c + 1) * TA]),
                             yc=np.ascontiguousarray(y_cv[c * TA:(c + 1) * TA]), **common) for c in range(NCORES)])
        xs = [np.asarray(r["xo"]) for r in r1]
        del r1, y_attn, y_lru, y_cv
        common = dict(w1=np.ascontiguousarray(inp["mlp_w1"][l]), w2=np.ascontiguousarray(inp["mlp_w2"][l]),
                      gcol=np.ascontiguousarray(inp["mlp_norm"][l].reshape(8, 128).T), ident=ca["ident"])
        r2 = _run(build_phase_c2(), [dict(x=np.ascontiguousarray(xs[c]), **common) for c in range(NCORES)])
        xs = [np.asarray(r["xo"]) for r in r2]
        del r2
    return np.concatenate(xs, axis=0).reshape(NB, S, D).astype(np.float32)
```

```python
import numpy as np
import ml_dtypes
from contextlib import ExitStack

import concourse.bass as bass
import concourse.mybir as mybir
from concourse.bass_utils import run_bass_kernel_spmd

F32 = mybir.dt.float32
BF16 = mybir.dt.bfloat16
AF = mybir.ActivationFunctionType
ALU = mybir.AluOpType
AX = mybir.AxisListType
NPBF = ml_dtypes.bfloat16

NCORES = 8
D = 1024
S = 16384
NB = 2
DEPTH = 2
N_IN = 2328
EPS = 1e-6
SAME_ENGINE_SYNC = True


class Buf:
    __slots__ = ("name", "w", "r", "dsem", "dcnt")

    def __init__(self, name):
        self.name = name
        self.w = None
        self.r = []
        self.dsem = None
        self.dcnt = 0


class Prog:
    ENG = ("pe", "act", "dve", "pool", "sp")

    def __init__(self, nc, stack):
        self.nc = nc
        self.stack = stack
        self.sem = {k: stack.enter_context(nc.semaphore("sem_" + k)) for k in ("pe", "act", "dve", "pool")}
        self.cnt = {k: 0 for k in self.sem}
        self.lists = {k: [] for k in self.ENG}
        self.waited = {k: {} for k in self.ENG}
        self.dsems = {}
        self.out_events = []
        self.nbuf = 0

    def buf(self, name=None):
        self.nbuf += 1
        return Buf(name or "b%d" % self.nbuf)

    def _waits(self, eng, reads, writes):
        deps = {}

        def add(ev):
            if ev is None:
                return
            k, v = ev
            if deps.get(k, 0) < v:
                deps[k] = v

        for b in reads:
            add(b.w)
        for b in writes:
            add(b.w)
            for ev in b.r:
                add(ev)
        out = []
        for k, v in deps.items():
            if k == eng and (eng == "pe" or not SAME_ENGINE_SYNC):
                continue
            if self.waited[eng].get(k, 0) >= v:
                continue
            self.waited[eng][k] = v
            out.append((k, v))
        return out

    def _semof(self, k):
        return self.sem[k] if isinstance(k, str) else self.dsems[k]

    def op(self, eng, fn, reads=(), writes=()):
        waits = [(self._semof(k), v) for k, v in self._waits(eng, reads, writes)]
        self.cnt[eng] += 1
        ev = (eng, self.cnt[eng])
        sem = self.sem[eng]

        def emit(e):
            for s, v in waits:
                e.wait_ge(s, v)
            fn(e).then_inc(sem, 1)

        self.lists[eng].append(emit)
        for b in reads:
            b.r.append(ev)
        for b in writes:
            b.w = ev
            b.r = []

    def dma(self, q, out, in_, reads=(), writes=(), is_output=False, **kw):
        owner = writes[0] if writes else reads[0]
        if owner.dsem is None:
            owner.dsem = ("d", len(self.dsems))
            self.dsems[owner.dsem] = self.stack.enter_context(self.nc.semaphore("dsem%d" % len(self.dsems)))
        waits = [(self._semof(k), v) for k, v in self._waits(q, reads, writes)]
        owner.dcnt += 16
        ev = (owner.dsem, owner.dcnt)
        sem = self.dsems[owner.dsem]

        def emit(e):
            for s, v in waits:
                e.wait_ge(s, v)
            e.dma_start(out=out, in_=in_, **kw).then_inc(sem, 16)

        self.lists[q].append(emit)
        for b in reads:
            b.r.append(ev)
        for b in writes:
            b.w = ev
            b.r = []
        if is_output:
            self.out_events.append(ev)

    def finish(self):
        fin = {}
        for k, v in self.out_events:
            fin[k] = max(fin.get(k, 0), v)
        waits = [(self._semof(k), v) for k, v in fin.items()]

        def emit(e):
            for s, v in waits:
                e.wait_ge(s, v)

        self.lists["sp"].append(emit)
        lists = self.lists
        with self.nc.Block() as block:
            @block.sync
            def _(e):
                for f in lists["sp"]:
                    f(e)

            @block.tensor
            def _(e):
                for f in lists["pe"]:
                    f(e)

            @block.scalar
            def _(e):
                for f in lists["act"]:
                    f(e)

            @block.vector
            def _(e):
                for f in lists["dve"]:
                    f(e)

            @block.gpsimd
            def _(e):
                for f in lists["pool"]:
                    f(e)


class Ctx:
    def __init__(self, name):
        self.nc = bass.Bass("TRN2", target_bir_lowering=False)
        self.stack = ExitStack()
        self.p = Prog(self.nc, self.stack)
        self.n = 0

    def dram(self, name, shape, dt, kind):
        return self.nc.dram_tensor(name, list(shape), dt, kind=kind).ap()

    def sb(self, shape, dt, name=None):
        self.n += 1
        return self.stack.enter_context(self.nc.sbuf_tensor(name or "sb%d" % self.n, list(shape), dt))

    def ps(self, shape, dt, name=None):
        self.n += 1
        return self.stack.enter_context(self.nc.psum_tensor(name or "ps%d" % self.n, list(shape), dt))


def _run(ctx, in_maps, keep=False):
    if not getattr(ctx, "finished", False):
        ctx.p.finish()
        ctx.finished = True
    res = run_bass_kernel_spmd(ctx.nc, in_maps, core_ids=list(range(NCORES)))
    if not keep:
        ctx.stack.close()
    return res.results


class Ring:
    def __init__(self, ctx, n, shape, dt, name, psum=False):
        self.t = [(ctx.ps if psum else ctx.sb)(shape, dt, "%s%d" % (name, i)) for i in range(n)]
        self.b = [ctx.p.buf("%s%d" % (name, i)) for i in range(n)]
        self.i = -1
        self.n = n

    def next(self):
        self.i = (self.i + 1) % self.n
        return self.t[self.i], self.b[self.i]


def load_weight_scaled(ctx, w_dram, K, N, gcol_t, gcol_b, wb, wb_b, stage, q="sp", col0=0):
    p = ctx.p
    for kc in range(K // 128):
        st, stb = stage.next()
        p.dma(q, st[:, 0:N], w_dram[kc * 128:(kc + 1) * 128, col0:col0 + N], writes=[stb])
        if gcol_t is None:
            p.op("dve", lambda e, st=st, kc=kc: e.tensor_copy(out=wb[:, kc, 0:N], in_=st[:, 0:N]),
                 reads=[stb], writes=[wb_b])
        else:
            p.op("dve", lambda e, st=st, kc=kc: e.tensor_scalar(
                out=wb[:, kc, 0:N], in0=st[:, 0:N], scalar1=gcol_t[:, kc:kc + 1], scalar2=None, op0=ALU.mult),
                reads=[stb, gcol_b], writes=[wb_b])


def rms_rstd(ctx, xt, xb, width, junk, junk_b, small, small_b):
    p = ctx.p
    p.op("act", lambda e: e.activation(out=junk[:, 0:width], in_=xt, func=AF.Square, accum_out=small[:, 0:1]),
         reads=[xb], writes=[junk_b, small_b])
    p.op("dve", lambda e: e.tensor_scalar(out=small[:, 1:2], in0=small[:, 0:1], scalar1=1.0 / width, scalar2=EPS,
                                          op0=ALU.mult, op1=ALU.add), reads=[small_b], writes=[small_b])
    p.op("act", lambda e: e.activation(out=small[:, 2:3], in_=small[:, 1:2], func=AF.Sqrt),
         reads=[small_b], writes=[small_b])
    p.op("dve", lambda e: e.reciprocal(out=small[:, 3:4], in_=small[:, 2:3]), reads=[small_b], writes=[small_b])


TA = 4096
A_CHUNKS = (
    [(c * 128, 128, "nq", c * 128) for c in range(4)]
    + [(512, 128, "raw", 512), (640, 128, "raw", 640), (768, 128, "nk1", 768), (896, 128, "raw", 896),
       (1024, 128, "nk2", 1024), (1152, 128, "raw", 1152)]
    + [(1280, 24, "gate", 0)]
    + [(1304, 128, "rawf", 0), (1432, 128, "rawf", 128), (1560, 128, "gelu", 256), (1688, 128, "gelu", 384)]
    + [(2072, 128, "glu_g", 0), (2200, 128, "glu_g", 1), (1816, 128, "glu_a", 0), (1944, 128, "glu_a", 1)]
)


def build_phase_a(ngroups=TA // 512):
    ctx = Ctx("phA")
    p = ctx.p
    T = ngroups * 512
    x = ctx.dram("x", [T, D], F32, "ExternalInput")
    w = ctx.dram("w", [D, N_IN], F32, "ExternalInput")
    gcol_d = ctx.dram("gcol", [128, 8], F32, "ExternalInput")
    gq_d = ctx.dram("gq", [128, 3], F32, "ExternalInput")
    bo_d = ctx.dram("blockones", [128, 128], F32, "ExternalInput")
    id_d = ctx.dram("ident", [128, 128], BF16, "ExternalInput")
    fa = ctx.dram("fa", [1280, T], BF16, "ExternalOutput")
    gat = ctx.dram("gat", [24, T], F32, "ExternalOutput")
    fl = ctx.dram("fl", [768, T], F32, "ExternalOutput")

    gcol, gcol_b = ctx.sb([128, 8], F32), p.buf()
    gq, gq_b = ctx.sb([128, 3], F32), p.buf()
    bo, bo_b = ctx.sb([128, 128], F32), p.buf()
    ident, id_b = ctx.sb([128, 128], BF16), p.buf()
    p.dma("sp", gcol[:, :], gcol_d[:, :], writes=[gcol_b])
    p.dma("sp", gq[:, :], gq_d[:, :], writes=[gq_b])
    p.dma("sp", bo[:, :], bo_d[:, :], writes=[bo_b])
    p.dma("sp", ident[:, :], id_d[:, :], writes=[id_b])

    wb, wb_b = ctx.sb([128, 8, N_IN], BF16), p.buf()
    stage = Ring(ctx, 2, [128, N_IN], F32, "wst")
    load_weight_scaled(ctx, w, D, N_IN, gcol, gcol_b, wb, wb_b, stage)

    xr = Ring(ctx, 2, [128, D], F32, "xt")
    junk, junk_b = ctx.sb([128, D], BF16), p.buf()
    small = Ring(ctx, 2, [128, 4], F32, "small")
    xn = Ring(ctx, 2, [128, D], BF16, "xn")
    pT = Ring(ctx, 1, [128, 8, 128], BF16, "pT", psum=True)
    hn = Ring(ctx, 2, [128, 8, 512], BF16, "hnT")
    pz = Ring(ctx, 3, [128, 512], F32, "pz", psum=True)
    pss = Ring(ctx, 2, [128, 512], F32, "pss", psum=True)
    sqv = Ring(ctx, 2, [128, 512], F32, "sqv")
    srr = Ring(ctx, 2, [128, 512], F32, "srr")
    ost = Ring(ctx, 3, [128, 512], BF16, "ost")
    osf = Ring(ctx, 3, [128, 512], F32, "osf")
    sg_t = [ctx.sb([128, 512], F32) for _ in range(2)]
    sg_b = [p.buf() for _ in range(2)]

    for g in range(ngroups):
        hT, hb = hn.next()
        for tt in range(4):
            r0 = g * 512 + tt * 128
            xt, xb = xr.next()
            p.dma("sp", xt[:, :], x[r0:r0 + 128, :], writes=[xb])
            sm, smb = small.next()
            rms_rstd(ctx, xt[:, :], xb, D, junk, junk_b, sm, smb)
            xnt, xnb = xn.next()
            p.op("dve", lambda e, xnt=xnt, xt=xt, sm=sm: e.tensor_scalar(
                out=xnt[:, :], in0=xt[:, :], scalar1=sm[:, 3:4], scalar2=None, op0=ALU.mult),
                reads=[xb, smb], writes=[xnb])
            pt, ptb = pT.next()
            for kc in range(8):
                p.op("pe", lambda e, pt=pt, xnt=xnt, kc=kc: e.transpose(
                    out=pt[:, kc, :], in_=xnt[:, kc * 128:(kc + 1) * 128], identity=ident[:, :]),
                    reads=[xnb, id_b], writes=[ptb])
            p.op("act", lambda e, hT=hT, pt=pt, tt=tt: e.copy(out=hT[:, :, tt * 128:(tt + 1) * 128], in_=pt[:, :, :]),
                 reads=[ptb], writes=[hb])
        c0 = g * 512
        for (col0, M, kind, orow) in A_CHUNKS:
            z, zb = pz.next()
            for kc in range(8):
                p.op("pe", lambda e, z=z, kc=kc, col0=col0, M=M, hT=hT: e.matmul(
                    out=z[0:M, :], lhsT=wb[:, kc, col0:col0 + M], rhs=hT[:, kc, :], start=(kc == 0), stop=(kc == 7)),
                    reads=[wb_b, hb], writes=[zb])
            if kind in ("nq", "nk1", "nk2"):
                gi = {"nq": 0, "nk1": 1, "nk2": 2}[kind]
                sc, bi = (1.0, 64.0 * EPS) if kind == "nq" else (1.0 / 64.0, EPS)
                sq, sqb = sqv.next()
                p.op("act", lambda e, sq=sq, z=z: e.activation(out=sq[:, :], in_=z[:, :], func=AF.Square),
                     reads=[zb], writes=[sqb])
                ss, ssb = pss.next()
                p.op("pe", lambda e, ss=ss, sq=sq: e.matmul(out=ss[:, :], lhsT=bo[:, :], rhs=sq[:, :],
                                                            start=True, stop=True),
                     reads=[bo_b, sqb], writes=[ssb])
                sr, srb = srr.next()
                p.op("act", lambda e, sr=sr, ss=ss, sc=sc, bi=bi: e.activation(
                    out=sr[:, :], in_=ss[:, :], func=AF.Sqrt, scale=sc, bias=bi), reads=[ssb], writes=[srb])
                p.op("dve", lambda e, sr=sr: e.reciprocal(out=sr[:, :], in_=sr[:, :]), reads=[srb], writes=[srb])
                o, ob = ost.next()
                p.op("dve", lambda e, o=o, z=z, sr=sr, gi=gi: e.scalar_tensor_tensor(
                    out=o[:, :], in0=z[:, :], scalar=gq[:, gi:gi + 1], in1=sr[:, :], op0=ALU.mult, op1=ALU.mult),
                    reads=[zb, srb, gq_b], writes=[ob])
                p.dma("pool", fa[orow:orow + 128, c0:c0 + 512], o[:, :], reads=[ob], is_output=True)
            elif kind == "raw":
                o, ob = ost.next()
                p.op("act", lambda e, o=o, z=z: e.copy(out=o[:, :], in_=z[:, :]), reads=[zb], writes=[ob])
                p.dma("pool", fa[orow:orow + 128, c0:c0 + 512], o[:, :], reads=[ob], is_output=True)
            elif kind == "gate":
                o, ob = osf.next()
                p.op("act", lambda e, o=o, z=z: e.activation(out=o[0:24, :], in_=z[0:24, :], func=AF.Sigmoid),
                     reads=[zb], writes=[ob])
                p.dma("pool", gat[0:24, c0:c0 + 512], o[0:24, :], reads=[ob], is_output=True)
            elif kind == "rawf":
                o, ob = osf.next()
                p.op("dve", lambda e, o=o, z=z: e.tensor_copy(out=o[:, :], in_=z[:, :]), reads=[zb], writes=[ob])
                p.dma("pool", fl[orow:orow + 128, c0:c0 + 512], o[:, :], reads=[ob], is_output=True)
            elif kind == "gelu":
                o, ob = osf.next()
                p.op("act", lambda e, o=o, z=z: e.activation(out=o[:, :], in_=z[:, :], func=AF.Gelu_apprx_tanh),
                     reads=[zb], writes=[ob])
                p.dma("pool", fl[orow:orow + 128, c0:c0 + 512], o[:, :], reads=[ob], is_output=True)
            elif kind == "glu_g":
                p.op("act", lambda e, z=z, orow=orow: e.activation(out=sg_t[orow][:, :], in_=z[:, :], func=AF.Sigmoid),
                     reads=[zb], writes=[sg_b[orow]])
            elif kind == "glu_a":
                o, ob = osf.next()
                p.op("dve", lambda e, o=o, z=z, orow=orow: e.tensor_tensor(
                    out=o[:, :], in0=z[:, :], in1=sg_t[orow][:, :], op=ALU.mult),
                    reads=[zb, sg_b[orow]], writes=[ob])
                p.dma("pool", fl[512 + orow * 128:512 + (orow + 1) * 128, c0:c0 + 512], o[:, :], reads=[ob],
                      is_output=True)
    return ctx


def consts_a():
    bo = np.zeros((128, 128), np.float32)
    bo[:64, :64] = 1.0
    bo[64:, 64:] = 1.0
    return {"blockones": bo, "ident": np.eye(128, dtype=np.float32).astype(NPBF)}


def run_phase_a(xs, w, attn_norm, q_norm, k_norm, ngroups=TA // 512):
    ctx = build_phase_a(ngroups)
    gq = np.stack([np.tile(q_norm, 2), np.tile(k_norm[1], 2), np.tile(k_norm[2], 2)], axis=1).astype(np.float32)
    common = dict(w=np.ascontiguousarray(w), gcol=np.ascontiguousarray(attn_norm.reshape(8, 128).T),
                  gq=np.ascontiguousarray(gq), **consts_a())
    in_maps = [dict(x=np.ascontiguousarray(xs[c]), **common) for c in range(NCORES)]
    return _run(ctx, in_maps)


def build_phase_c1(ntiles=TA // 128):
    ctx = Ctx("phC1")
    p = ctx.p
    T = ntiles * 128
    x = ctx.dram("x", [T, D], F32, "ExternalInput")
    ya = ctx.dram("ya", [T, 512], F32, "ExternalInput")
    yl = ctx.dram("yl", [T, 256], F32, "ExternalInput")
    yc = ctx.dram("yc", [T, 256], F32, "ExternalInput")
    ln_d = ctx.dram("lnrep", [128, 512], F32, "ExternalInput")
    w = ctx.dram("w", [D, D], F32, "ExternalInput")
    gcol_d = ctx.dram("gcol", [128, 8], F32, "ExternalInput")
    id_d = ctx.dram("ident", [128, 128], BF16, "ExternalInput")
    xo = ctx.dram("xo", [T, D], F32, "ExternalOutput")

    gcol, gcol_b = ctx.sb([128, 8], F32), p.buf()
    ln, ln_b = ctx.sb([128, 512], F32), p.buf()
    ident, id_b = ctx.sb([128, 128], BF16), p.buf()
    p.dma("sp", gcol[:, :], gcol_d[:, :], writes=[gcol_b])
    p.dma("sp", ln[:, :], ln_d[:, :], writes=[ln_b])
    p.dma("sp", ident[:, :], id_d[:, :], writes=[id_b])
    wb, wb_b = ctx.sb([128, 8, D], BF16), p.buf()
    stage = Ring(ctx, 2, [128, D], F32, "wst")
    load_weight_scaled(ctx, w, D, D, gcol, gcol_b, wb, wb_b, stage)

    xr = Ring(ctx, 2, [128, D], F32, "xt")
    yar = Ring(ctx, 2, [128, 512], F32, "ya")
    ylr = Ring(ctx, 2, [128, 256], F32, "yl")
    ycr = Ring(ctx, 2, [128, 256], F32, "yc")
    junk, junk_b = ctx.sb([128, D], BF16), p.buf()
    st6 = Ring(ctx, 2, [128, 8], F32, "st6")
    sm_a = Ring(ctx, 2, [128, 4], F32, "sma")
    sm_l = Ring(ctx, 2, [128, 4], F32, "sml")
    sm_c = Ring(ctx, 2, [128, 4], F32, "smc")
    ycat = Ring(ctx, 2, [128, D], BF16, "ycat")
    pT = Ring(ctx, 2, [128, 8, 128], BF16, "pT", psum=True)
    yT = Ring(ctx, 2, [128, 8, 128], BF16, "yT")
    po = Ring(ctx, 4, [128, 512], F32, "po", psum=True)
    xo_r = Ring(ctx, 2, [128, D], F32, "xo")

    for t in range(ntiles):
        r0 = t * 128
        xt, xb = xr.next()
        p.dma("sp", xt[:, :], x[r0:r0 + 128, :], writes=[xb])
        a, ab = yar.next()
        p.dma("sp", a[:, :], ya[r0:r0 + 128, :], writes=[ab])
        l, lb = ylr.next()
        p.dma("sp", l[:, :], yl[r0:r0 + 128, :], writes=[lb])
        c, cb = ycr.next()
        p.dma("sp", c[:, :], yc[r0:r0 + 128, :], writes=[cb])
        s6, s6b = st6.next()
        p.op("dve", lambda e, s6=s6, c=c: e.bn_stats(out=s6[:, 0:6], in_=c[:, :]), reads=[cb], writes=[s6b])
        p.op("dve", lambda e, s6=s6: e.bn_aggr(out=s6[:, 6:8], in_=s6[:, 0:6]), reads=[s6b], writes=[s6b])
        p.op("dve", lambda e, s6=s6: e.tensor_scalar(out=s6[:, 0:1], in0=s6[:, 7:8], scalar1=EPS, scalar2=None,
                                                     op0=ALU.add), reads=[s6b], writes=[s6b])
        p.op("act", lambda e, s6=s6: e.activation(out=s6[:, 1:2], in_=s6[:, 0:1], func=AF.Sqrt),
             reads=[s6b], writes=[s6b])
        p.op("dve", lambda e, s6=s6: e.reciprocal(out=s6[:, 2:3], in_=s6[:, 1:2]), reads=[s6b], writes=[s6b])
        p.op("dve", lambda e, s6=s6, c=c: e.tensor_scalar(out=c[:, :], in0=c[:, :], scalar1=s6[:, 6:7],
                                                          scalar2=s6[:, 2:3], op0=ALU.subtract, op1=ALU.mult),
             reads=[cb, s6b], writes=[cb])
        p.op("dve", lambda e, c=c: e.tensor_tensor(out=c[:, :], in0=c[:, :], in1=ln[:, 0:256], op=ALU.mult),
             reads=[cb, ln_b], writes=[cb])
        p.op("dve", lambda e, c=c: e.tensor_tensor(out=c[:, :], in0=c[:, :], in1=ln[:, 256:512], op=ALU.add),
             reads=[cb, ln_b], writes=[cb])
        p.op("act", lambda e, c=c: e.activation(out=c[:, :], in_=c[:, :], func=AF.Silu), reads=[cb], writes=[cb])
        yct, ycb = ycat.next()
        for (src, sb_, ring, wdt, off) in ((a, ab, sm_a, 512, 0), (l, lb, sm_l, 256, 512), (c, cb, sm_c, 256, 768)):
            sm, smb = ring.next()
            rms_rstd(ctx, src[:, :], sb_, wdt, junk, junk_b, sm, smb)
            p.op("dve", lambda e, yct=yct, src=src, sm=sm, off=off, wdt=wdt: e.tensor_scalar(
                out=yct[:, off:off + wdt], in0=src[:, :], scalar1=sm[:, 3:4], scalar2=None, op0=ALU.mult),
                reads=[sb_, smb], writes=[ycb])
        pt, ptb = pT.next()
        for kc in range(8):
            p.op("pe", lambda e, pt=pt, yct=yct, kc=kc: e.transpose(
                out=pt[:, kc, :], in_=yct[:, kc * 128:(kc + 1) * 128], identity=ident[:, :]),
                reads=[ycb, id_b], writes=[ptb])
        yt, ytb = yT.next()
        p.op("act", lambda e, yt=yt, pt=pt: e.copy(out=yt[:, :, :], in_=pt[:, :, :]), reads=[ptb], writes=[ytb])
        o, ob = xo_r.next()
        for half in range(2):
            ps, psb = po.next()
            for kc in range(8):
                p.op("pe", lambda e, ps=ps, yt=yt, kc=kc, half=half: e.matmul(
                    out=ps[:, :], lhsT=yt[:, kc, :], rhs=wb[:, kc, half * 512:(half + 1) * 512],
                    start=(kc == 0), stop=(kc == 7)), reads=[ytb, wb_b], writes=[psb])
            p.op("dve", lambda e, o=o, ps=ps, xt=xt, half=half: e.tensor_tensor(
                out=o[:, half * 512:(half + 1) * 512], in0=ps[:, :], in1=xt[:, half * 512:(half + 1) * 512],
                op=ALU.add), reads=[psb, xb], writes=[ob])
        p.dma("pool", xo[r0:r0 + 128, :], o[:, :], reads=[ob], is_output=True)
    return ctx


def run_phase_c1(xs, yas, yls, ycs, w_out, out_norm, ln_g, ln_b, ntiles=TA // 128):
    ctx = build_phase_c1(ntiles)
    lnrep = np.ascontiguousarray(np.broadcast_to(np.concatenate([ln_g, ln_b])[None, :], (128, 512))).astype(np.float32)
    common = dict(w=np.ascontiguousarray(w_out), gcol=np.ascontiguousarray(out_norm.reshape(8, 128).T),
                  lnrep=lnrep, ident=consts_a()["ident"])
    in_maps = [dict(x=np.ascontiguousarray(xs[c]), ya=np.ascontiguousarray(yas[c]), yl=np.ascontiguousarray(yls[c]),
                    yc=np.ascontiguousarray(ycs[c]), **common) for c in range(NCORES)]
    return [r["xo"] for r in _run(ctx, in_maps)]


def build_phase_c2(ngroups=TA // 256):
    ctx = Ctx("phC2")
    p = ctx.p
    T = ngroups * 256
    DF = 4 * D
    x = ctx.dram("x", [T, D], F32, "ExternalInput")
    w1 = ctx.dram("w1", [D, DF], F32, "ExternalInput")
    w2 = ctx.dram("w2", [DF, D], F32, "ExternalInput")
    gcol_d = ctx.dram("gcol", [128, 8], F32, "ExternalInput")
    id_d = ctx.dram("ident", [128, 128], BF16, "ExternalInput")
    xo = ctx.dram("xo", [T, D], F32, "ExternalOutput")

    gcol, gcol_b = ctx.sb([128, 8], F32), p.buf()
    ident, id_b = ctx.sb([128, 128], BF16), p.buf()
    p.dma("sp", gcol[:, :], gcol_d[:, :], writes=[gcol_b])
    p.dma("sp", ident[:, :], id_d[:, :], writes=[id_b])
    w1b, w1b_b = ctx.sb([128, 8, DF], BF16), p.buf()
    w2b, w2b_b = ctx.sb([128, 32, D], BF16), p.buf()
    stage = Ring(ctx, 2, [128, D], F32, "wst")
    for kc in range(8):
        for cq in range(4):
            st, stb = stage.next()
            p.dma("sp", st[:, :], w1[kc * 128:(kc + 1) * 128, cq * D:(cq + 1) * D], writes=[stb])
            p.op("dve", lambda e, st=st, kc=kc, cq=cq: e.tensor_scalar(
                out=w1b[:, kc, cq * D:(cq + 1) * D], in0=st[:, :], scalar1=gcol[:, kc:kc + 1], scalar2=None,
                op0=ALU.mult), reads=[stb, gcol_b], writes=[w1b_b])
    for f in range(32):
        st, stb = stage.next()
        p.dma("sp", st[:, :], w2[f * 128:(f + 1) * 128, :], writes=[stb])
        p.op("pool", lambda e, st=st, f=f: e.tensor_copy(out=w2b[:, f, :], in_=st[:, :]), reads=[stb], writes=[w2b_b])

    xm = Ring(ctx, 4, [128, D], F32, "xm")
    junk, junk_b = ctx.sb([128, D], BF16), p.buf()
    small = Ring(ctx, 2, [128, 4], F32, "small")
    hm = Ring(ctx, 2, [128, D], BF16, "hm")
    pT = Ring(ctx, 2, [128, 8, 128], BF16, "pT", psum=True)
    hmT = Ring(ctx, 2, [128, 8, 256], BF16, "hmT")
    ph = Ring(ctx, 3, [128, 256], F32, "ph", psum=True)
    rl = Ring(ctx, 3, [128, 256], F32, "rl")
    h1 = Ring(ctx, 1, [128, 32, 256], BF16, "h1T")
    po = Ring(ctx, 3, [128, 512], F32, "po", psum=True)

    for g in range(ngroups):
        hT, hTb = hmT.next()
        tiles = []
        for tt in range(2):
            r0 = g * 256 + tt * 128
            xt, xb = xm.next()
            tiles.append((xt, xb, r0))
            p.dma("sp", xt[:, :], x[r0:r0 + 128, :], writes=[xb])
            sm, smb = small.next()
            rms_rstd(ctx, xt[:, :], xb, D, junk, junk_b, sm, smb)
            h, hb = hm.next()
            p.op("dve", lambda e, h=h, xt=xt, sm=sm: e.tensor_scalar(
                out=h[:, :], in0=xt[:, :], scalar1=sm[:, 3:4], scalar2=None, op0=ALU.mult),
                reads=[xb, smb], writes=[hb])
            pt, ptb = pT.next()
            for kc in range(8):
                p.op("pe", lambda e, pt=pt, h=h, kc=kc: e.transpose(
                    out=pt[:, kc, :], in_=h[:, kc * 128:(kc + 1) * 128], identity=ident[:, :]),
                    reads=[hb, id_b], writes=[ptb])
            p.op("act", lambda e, hT=hT, pt=pt, tt=tt: e.copy(out=hT[:, :, tt * 128:(tt + 1) * 128], in_=pt[:, :, :]),
                 reads=[ptb], writes=[hTb])
        h1t, h1b = h1.next()
        for f in range(32):
            ps, psb = ph.next()
            for kc in range(8):
                p.op("pe", lambda e, ps=ps, kc=kc, f=f, hT=hT: e.matmul(
                    out=ps[:, :], lhsT=w1b[:, kc, f * 128:(f + 1) * 128], rhs=hT[:, kc, :],
                    start=(kc == 0), stop=(kc == 7)), reads=[w1b_b, hTb], writes=[psb])
            r, rb = rl.next()
            p.op("act", lambda e, r=r, ps=ps: e.activation(out=r[:, :], in_=ps[:, :], func=AF.Relu),
                 reads=[psb], writes=[rb])
            p.op("dve", lambda e, r=r, ps=ps, f=f, h1t=h1t: e.tensor_tensor(
                out=h1t[:, f, :], in0=ps[:, :], in1=r[:, :], op=ALU.mult), reads=[psb, rb], writes=[h1b])
        for tt in range(2):
            xt, xb, r0 = tiles[tt]
            for half in range(2):
                ps, psb = po.next()
                for f in range(32):
                    p.op("pe", lambda e, ps=ps, f=f, tt=tt, half=half, h1t=h1t: e.matmul(
                        out=ps[:, :], lhsT=h1t[:, f, tt * 128:(tt + 1) * 128],
                        rhs=w2b[:, f, half * 512:(half + 1) * 512], start=(f == 0), stop=(f == 31)),
                        reads=[h1b, w2b_b], writes=[psb])
                p.op("dve", lambda e, ps=ps, xt=xt, half=half: e.tensor_tensor(
                    out=xt[:, half * 512:(half + 1) * 512], in0=ps[:, :], in1=xt[:, half * 512:(half + 1) * 512],
                    op=ALU.add), reads=[psb, xb], writes=[xb])
            p.dma("pool", xo[r0:r0 + 128, :], xt[:, :], reads=[xb], is_output=True)
    return ctx


def run_phase_c2(xs, w1, w2, mlp_norm, ngroups=TA // 256):
    ctx = build_phase_c2(ngroups)
    common = dict(w1=np.ascontiguousarray(w1), w2=np.ascontiguousarray(w2),
                  gcol=np.ascontiguousarray(mlp_norm.reshape(8, 128).T), ident=consts_a()["ident"])
    in_maps = [dict(x=np.ascontiguousarray(xs[c]), **common) for c in range(NCORES)]
    return [r["xo"] for r in _run(ctx, in_maps)]


SEQ_CH = 2048


def build_phase_bseq(nchunks=S // SEQ_CH):
    ctx = Ctx("phBseq")
    p = ctx.p
    T = nchunks * SEQ_CH
    C = 64
    lx = ctx.dram("lx", [C, T], F32, "ExternalInput")
    lg = ctx.dram("lg", [C, T], F32, "ExternalInput")
    cv = ctx.dram("cv", [C, T], F32, "ExternalInput")
    par_d = ctx.dram("par", [C, 40], F32, "ExternalInput")
    bd_d = ctx.dram("bd", [C, 2, C], F32, "ExternalInput")
    id_d = ctx.dram("ident64", [C, C], F32, "ExternalInput")
    yl = ctx.dram("ylT", [C, T], F32, "ExternalOutput")
    yc = ctx.dram("ycT", [C, T], F32, "ExternalOutput")

    par, par_b = ctx.sb([C, 40], F32), p.buf()
    bdf, bdf_b = ctx.sb([C, 2, C], F32), p.buf()
    idf, idf_b = ctx.sb([C, C], F32), p.buf()
    p.dma("sp", par[:, :], par_d[:, :], writes=[par_b])
    p.dma("sp", bdf[:, :, :], bd_d[:, :, :], writes=[bdf_b])
    p.dma("sp", idf[:, :], id_d[:, :], writes=[idf_b])
    bd, bd_b = ctx.sb([C, 2, C], BF16), p.buf()
    p.op("dve", lambda e: e.tensor_copy(out=bd[:, :, :], in_=bdf[:, :, :]), reads=[bdf_b], writes=[bd_b])
    dg, dg_b = ctx.sb([C, 31, C], BF16), p.buf()
    for k in range(31):
        p.op("dve", lambda e, k=k: e.tensor_scalar(out=dg[:, k, :], in0=idf[:, :], scalar1=par[:, 9 + k:10 + k],
                                                   scalar2=None, op0=ALU.mult), reads=[idf_b, par_b], writes=[dg_b])
    cc, cc_b = ctx.sb([C, 4], F32), p.buf()
    p.op("act", lambda e: e.activation(out=cc[:, 0:1], in_=par[:, 7:8], func=AF.Exp, scale=-1.0),
         reads=[par_b], writes=[cc_b])
    p.op("act", lambda e: e.activation(out=cc[:, 1:2], in_=cc[:, 0:1], func=AF.Ln, bias=1.0),
         reads=[cc_b], writes=[cc_b])
    p.op("dve", lambda e: e.tensor_scalar(out=cc[:, 2:3], in0=cc[:, 1:2], scalar1=-8.0, scalar2=None, op0=ALU.mult),
         reads=[cc_b], writes=[cc_b])

    W = SEQ_CH
    lxh = Ring(ctx, 2, [C, 3 + W], F32, "lxh")
    lgr = Ring(ctx, 2, [C, W], F32, "lg")
    cvf = Ring(ctx, 2, [C, 30 + W], F32, "cvf")
    cvb = Ring(ctx, 2, [C, 30 + W], BF16, "cvb")
    xr_r = Ring(ctx, 2, [C, W], F32, "xr")
    xrb_r = Ring(ctx, 2, [C, W], BF16, "xrb")
    r_r = Ring(ctx, 2, [C, W], F32, "r")
    ig_r = Ring(ctx, 2, [C, W], F32, "ig")
    a_r = Ring(ctx, 2, [C, W], F32, "a")
    m_r = Ring(ctx, 2, [C, W], F32, "m")
    h_r = Ring(ctx, 2, [C, W], F32, "h")
    yo_r = Ring(ctx, 2, [C, W], F32, "yo")
    co_r = Ring(ctx, 2, [C, W], F32, "co")
    pg = Ring(ctx, 4, [C, 512], F32, "pg", psum=True)
    pc = Ring(ctx, 3, [C, 512], F32, "pc", psum=True)
    hprev = None

    for ch in range(nchunks):
        c0 = ch * W
        xh, xhb = lxh.next()
        if ch == 0:
            p.op("pool", lambda e, xh=xh: e.memset(xh[:, 0:3], 0.0), writes=[xhb])
            p.dma("sp", xh[:, 3:3 + W], lx[:, 0:W], writes=[xhb])
        else:
            p.dma("sp", xh[:, :], lx[:, c0 - 3:c0 + W], writes=[xhb])
        g, gb = lgr.next()
        p.dma("sp", g[:, :], lg[:, c0:c0 + W], writes=[gb])
        xr, xrb_ = xr_r.next()
        p.op("dve", lambda e, xr=xr, xh=xh: e.tensor_scalar(out=xr[:, :], in0=xh[:, 3:3 + W], scalar1=par[:, 3:4],
                                                            scalar2=par[:, 4:5], op0=ALU.mult, op1=ALU.add),
             reads=[xhb, par_b], writes=[xrb_])
        for k in range(3):
            p.op("dve", lambda e, xr=xr, xh=xh, k=k: e.scalar_tensor_tensor(
                out=xr[:, :], in0=xh[:, k:k + W], scalar=par[:, k:k + 1], in1=xr[:, :], op0=ALU.mult, op1=ALU.add),
                reads=[xhb, par_b, xrb_], writes=[xrb_])
        xb16, xb16b = xrb_r.next()
        p.op("act", lambda e, xb16=xb16, xr=xr: e.copy(out=xb16[:, :], in_=xr[:, :]), reads=[xrb_], writes=[xb16b])
        r, rb = r_r.next()
        ig, igb = ig_r.next()
        for (dst, dstb, wi, bcol) in ((r, rb, 0, 5), (ig, igb, 1, 6)):
            for ct in range(W // 512):
                ps, psb = pg.next()
                p.op("pe", lambda e, ps=ps, wi=wi, ct=ct, xb16=xb16: e.matmul(
                    out=ps[:, :], lhsT=bd[:, wi, :], rhs=xb16[:, ct * 512:(ct + 1) * 512], start=True, stop=True),
                    reads=[bd_b, xb16b], writes=[psb])
                p.op("act", lambda e, ps=ps, dst=dst, ct=ct, bcol=bcol: e.activation(
                    out=dst[:, ct * 512:(ct + 1) * 512], in_=ps[:, :], func=AF.Sigmoid, bias=par[:, bcol:bcol + 1]),
                    reads=[psb, par_b], writes=[dstb])
        a, ab = a_r.next()
        p.op("act", lambda e, a=a, r=r: e.activation(out=a[:, :], in_=r[:, :], func=AF.Exp, scale=cc[:, 2:3]),
             reads=[rb, cc_b], writes=[ab])
        m, mb = m_r.next()
        p.op("pool", lambda e, m=m, a=a: e.tensor_tensor(out=m[:, :], in0=a[:, :], in1=a[:, :], op=ALU.mult),
             reads=[ab], writes=[mb])
        p.op("act", lambda e, m=m: e.activation(out=m[:, :], in_=m[:, :], func=AF.Sqrt, scale=-1.0, bias=1.0),
             reads=[mb], writes=[mb])
        p.op("pool", lambda e, ig=ig, xr=xr: e.tensor_tensor(out=ig[:, :], in0=ig[:, :], in1=xr[:, :], op=ALU.mult),
             reads=[igb, xrb_], writes=[igb])
        p.op("dve", lambda e, ig=ig, m=m: e.tensor_tensor(out=ig[:, :], in0=ig[:, :], in1=m[:, :], op=ALU.mult),
             reads=[igb, mb], writes=[igb])
        h, hb = h_r.next()
        if hprev is None:
            p.op("dve", lambda e, h=h, a=a, ig=ig: e.tensor_tensor_scan(
                out=h[:, :], data0=a[:, :], data1=ig[:, :], initial=0.0, op0=ALU.mult, op1=ALU.add),
                reads=[ab, igb], writes=[hb])
        else:
            hp, hpb = hprev
            p.op("dve", lambda e, h=h, a=a, ig=ig, hp=hp: e.tensor_tensor_scan(
                out=h[:, :], data0=a[:, :], data1=ig[:, :], initial=hp[:, W - 1:W], op0=ALU.mult, op1=ALU.add),
                reads=[ab, igb, hpb], writes=[hb])
        hprev = (h, hb)
        yo, yob = yo_r.next()
        p.op("pool", lambda e, yo=yo, h=h, g=g: e.tensor_tensor(out=yo[:, :], in0=h[:, :], in1=g[:, :], op=ALU.mult),
             reads=[hb, gb], writes=[yob])
        p.dma("pool", yl[:, c0:c0 + W], yo[:, :], reads=[yob], is_output=True)
        cf, cfb = cvf.next()
        if ch == 0:
            p.op("pool", lambda e, cf=cf: e.memset(cf[:, 0:30], 0.0), writes=[cfb])
            p.dma("sp", cf[:, 30:30 + W], cv[:, 0:W], writes=[cfb])
        else:
            p.dma("sp", cf[:, :], cv[:, c0 - 30:c0 + W], writes=[cfb])
        cb16, cb16b = cvb.next()
        p.op("act", lambda e, cb16=cb16, cf=cf: e.copy(out=cb16[:, :], in_=cf[:, :]), reads=[cfb], writes=[cb16b])
        co, cob = co_r.next()
        for ct in range(W // 512):
            ps, psb = pc.next()
            for k in range(31):
                p.op("pe", lambda e, ps=ps, k=k, ct=ct, cb16=cb16: e.matmul(
                    out=ps[:, :], lhsT=dg[:, k, :], rhs=cb16[:, ct * 512 + k:ct * 512 + k + 512],
                    start=(k == 0), stop=(k == 30)), reads=[dg_b, cb16b], writes=[psb])
            p.op("dve", lambda e, ps=ps, co=co, ct=ct: e.tensor_scalar(
                out=co[:, ct * 512:(ct + 1) * 512], in0=ps[:, :], scalar1=par[:, 8:9], scalar2=None, op0=ALU.add),
                reads=[psb, par_b], writes=[cob])
        p.dma("pool", yc[:, c0:c0 + W], co[:, :], reads=[cob], is_output=True)
    return ctx


def bseq_params(inp, l, cc):
    sl = slice(64 * cc, 64 * cc + 64)
    par = np.zeros((64, 40), np.float32)
    par[:, 0:4] = inp["lru_conv_w"][l][:, sl].T
    par[:, 4] = inp["lru_conv_b"][l][sl]
    par[:, 5] = inp["lru_ba"][l][sl]
    par[:, 6] = inp["lru_bx"][l][sl]
    par[:, 7] = inp["lru_lambda"][l][sl]
    par[:, 8] = inp["cv_dw_b"][l][sl]
    par[:, 9:40] = inp["cv_dw_w"][l][:, sl].T
    bd = np.zeros((64, 2, 64), np.float32)
    for hh in range(2):
        bd[32 * hh:32 * hh + 32, 0, 32 * hh:32 * hh + 32] = inp["lru_wa"][l][2 * cc + hh]
        bd[32 * hh:32 * hh + 32, 1, 32 * hh:32 * hh + 32] = inp["lru_wx"][l][2 * cc + hh]
    return dict(par=par, bd=bd, ident64=np.eye(64, dtype=np.float32))


def run_phase_bseq(inp, l, lxs, lgs, cvs, nchunks=S // SEQ_CH):
    ctx = build_phase_bseq(nchunks)
    in_maps = [dict(lx=np.ascontiguousarray(lxs[c]), lg=np.ascontiguousarray(lgs[c]), cv=np.ascontiguousarray(cvs[c]),
                    **bseq_params(inp, l, c % 4)) for c in range(NCORES)]
    res = _run(ctx, in_maps)
    return [r["ylT"] for r in res], [r["ycT"] for r in res]


NEG = -1e30
LOOKAHEAD = 1
DBG_SKIP = set()
ATTN_SPLITS = ((0, 32),)


def build_phase_battn(NJ=32, J0=0):
    ctx = Ctx("phBattn")
    p = ctx.p
    qT_d = ctx.dram("qT", [128, NJ, 512], BF16, "ExternalInput")
    kcin_d = ctx.dram("kcin", [128, S], BF16, "ExternalInput")
    vcin_d = ctx.dram("vcin", [128, S], BF16, "ExternalInput")
    ksel_d = ctx.dram("kselT", [128, S], BF16, "ExternalInput")
    vsel_d = ctx.dram("vsel", [128, 128, 2, 65], BF16, "ExternalInput")
    kw_d = ctx.dram("kw", [128, NJ, 640], BF16, "ExternalInput")
    vw_d = ctx.dram("vw", [128, NJ, 5, 2, 65], BF16, "ExternalInput")
    gat_d = ctx.dram("gates", [128, NJ, 24], F32, "ExternalInput")
    w1_d = ctx.dram("w1rep", [2, 128, 32 * 128], F32, "ExternalInput")
    posT_d = ctx.dram("posT", [64, 2, 32], F32, "ExternalInput")
    w2h_d = ctx.dram("w2h", [128, 256], F32, "ExternalInput")
    w2v_d = ctx.dram("w2v", [128, 64], F32, "ExternalInput")
    kn_d = ctx.dram("kn0", [128, 1], F32, "ExternalInput")
    bo_d = ctx.dram("blockones", [128, 128], F32, "ExternalInput")
    id_d = ctx.dram("ident", [128, 128], BF16, "ExternalInput")
    thr_d = ctx.dram("thr", [128, 32], F32, "ExternalInput")
    iotc_d = ctx.dram("iotaC", [128, 1024], F32, "ExternalInput")
    ft_d = ctx.dram("FT", [128, 512], F32, "ExternalInput")
    cap_d = ctx.dram("CAPT", [128, 512], F32, "ExternalInput")
    c4_d = ctx.dram("causal4", [128, 4, 128], F32, "ExternalInput")
    wm_d = ctx.dram("wmask", [128, 2, 128], F32, "ExternalInput")
    eb_d = ctx.dram("Ebig", [128, 8192], BF16, "ExternalInput")
    yo_d = ctx.dram("yatt", [NJ, 128, 512], F32, "ExternalOutput")

    def const(shape, dt, src):
        t, b = ctx.sb(shape, dt), p.buf()
        full = tuple(slice(None) for _ in shape)
        p.dma("sp", t[full], src[full], writes=[b])
        return t, b

    big, big_b = ctx.sb([128, S], BF16), p.buf()
    p.dma("sp", big[:, :], kcin_d[:, :], writes=[big_b])
    kn, kn_b = const([128, 1], F32, kn_d)
    bo, bo_b = const([128, 128], F32, bo_d)
    ident, id_b = const([128, 128], BF16, id_d)
    thr, thr_b = const([128, 32], F32, thr_d)
    iotc, iotc_b = const([128, 1024], F32, iotc_d)
    FT, FT_b = const([128, 512], F32, ft_d)
    CAPT, CAP_b = const([128, 512], F32, cap_d)
    c4, c4_b = const([128, 4, 128], F32, c4_d)
    wm, wm_b = const([128, 2, 128], F32, wm_d)
    Ebig, eb_b = const([128, 8192], BF16, eb_d)
    gat, gat_b = const([128, NJ, 24], F32, gat_d)
    vsel, vsel_b = const([128, 128, 2, 65], BF16, vsel_d)
    posf, posf_b = const([64, 2, 32], F32, posT_d)
    w2hf, w2hf_b = const([128, 256], F32, w2h_d)
    w2vf, w2vf_b = const([128, 64], F32, w2v_d)
    posb, posb_b = ctx.sb([64, 2, 32], BF16), p.buf()
    w2h, w2h_b = ctx.sb([128, 2, 128], BF16), p.buf()
    w2v, w2v_b = ctx.sb([128, 64], BF16), p.buf()
    p.op("dve", lambda e: e.tensor_copy(out=posb[:, :, :], in_=posf[:, :, :]), reads=[posf_b], writes=[posb_b])
    p.op("dve", lambda e: e.tensor_copy(out=w2h[:, :, :], in_=w2hf[:, :].rearrange("p (h m) -> p h m", h=2)),
         reads=[w2hf_b], writes=[w2h_b])
    p.op("dve", lambda e: e.tensor_copy(out=w2v[:, :], in_=w2vf[:, :]), reads=[w2vf_b], writes=[w2v_b])

    pS = Ring(ctx, 3, [128, 512], F32, "pS", psum=True)
    pmx_r = Ring(ctx, 2, [128, 512], F32, "pmx", psum=True)
    acc = Ring(ctx, 2, [128, 4, 128], F32, "acc", psum=True)
    pT_t = ctx.ps([128, 8, 128], BF16, "pT")
    pT_b = [p.buf() for _ in range(8)]
    pTi = [0]
    pmi = [0]

    def next_pT():
        pTi[0] = (pTi[0] + 1) % 8
        return pT_t[:, pTi[0], :], pT_b[pTi[0]]

    def next_pmx():
        t, b = pmx_r.next()
        return t[:, 0:128], b

    kcT, kcT_b = ctx.sb([128, 1024], BF16), p.buf()
    vc, vc_b = ctx.sb([128, 8, 2, 65], BF16), p.buf()
    p.op("pool", lambda e: e.memset(kcT[:, :], 0.0), writes=[kcT_b])
    p.op("pool", lambda e: e.memset(vc[:, :, :, :], 1.0), writes=[vc_b])
    w1st, w1st_b = ctx.sb([128, 4096], F32), p.buf()
    w1r, w1r_b = ctx.sb([128, 32, 128], BF16), p.buf()
    hid = [ctx.sb([128, 1024], BF16) for _ in range(2)]
    hid_b = [p.buf() for _ in range(2)]
    b1, b1_b = ctx.sb([128, 2], F32), p.buf()
    sqv = Ring(ctx, 2, [128, 512], F32, "sqv")
    bigv = big[:, :].rearrange("p (n s) -> p n s", s=16)
    for kv in range(2):
        if kv == 1:
            p.dma("sp", big[:, :], vcin_d[:, :], writes=[big_b])
        p.dma("sp", w1st[:, :], w1_d[kv, :, :], writes=[w1st_b])
        p.op("dve", lambda e: e.tensor_copy(out=w1r[:, :, :], in_=w1st[:, :].rearrange("p (a b) -> p a b", a=32)),
             reads=[w1st_b], writes=[w1r_b])
        ps, psb = pS.next()
        for pp in range(32):
            p.op("pe", lambda e, ps=ps, pp=pp, kv=kv: e.matmul(
                out=ps[:, 0:1], lhsT=w1r[0:64, pp, :], rhs=posb[0:64, kv, pp:pp + 1], start=(pp == 0), stop=(pp == 31)),
                reads=[w1r_b, posb_b], writes=[psb])
        p.op("dve", lambda e, ps=ps, kv=kv: e.tensor_copy(out=b1[:, kv:kv + 1], in_=ps[:, 0:1]),
             reads=[psb], writes=[b1_b])
        for h in range(2):
            p.op("pool", lambda e, h=h: e.memset(hid[h][:, :], 0.0), writes=[hid_b[h]])
            for nt in range(2):
                N = 512 if nt == 0 else 511
                ps, psb = pS.next()
                for pp in range(32):
                    n0 = nt * 512 + pp // 16
                    p.op("pe", lambda e, ps=ps, pp=pp, h=h, n0=n0, N=N: e.matmul(
                        out=ps[:, 0:N], lhsT=w1r[h * 64:(h + 1) * 64, pp, :],
                        rhs=bigv[h * 64:(h + 1) * 64, n0:n0 + N, pp % 16], start=(pp == 0), stop=(pp == 31)),
                        reads=[w1r_b, big_b], writes=[psb])
                p.op("act", lambda e, ps=ps, h=h, nt=nt, N=N, kv=kv: e.activation(
                    out=hid[h][:, nt * 512:nt * 512 + N], in_=ps[:, 0:N], func=AF.Gelu_apprx_tanh,
                    bias=b1[:, kv:kv + 1]), reads=[psb, b1_b], writes=[hid_b[h]])
        if kv == 0:
            for nt in range(2):
                N = 512 if nt == 0 else 511
                ps, psb = pS.next()
                for h in range(2):
                    p.op("pe", lambda e, ps=ps, h=h, nt=nt, N=N: e.matmul(
                        out=ps[:, 0:N], lhsT=w2h[:, h, :], rhs=hid[h][:, nt * 512:nt * 512 + N],
                        start=(h == 0), stop=(h == 1)), reads=[w2h_b, hid_b[h]], writes=[psb])
                sq, sqb = sqv.next()
                p.op("act", lambda e, sq=sq, ps=ps, N=N: e.activation(out=sq[:, 0:N], in_=ps[:, 0:N], func=AF.Square),
                     reads=[psb], writes=[sqb])
                ss, ssb = pS.next()
                p.op("pe", lambda e, ss=ss, sq=sq, N=N: e.matmul(out=ss[:, 0:N], lhsT=bo[:, :], rhs=sq[:, 0:N],
                                                                 start=True, stop=True),
                     reads=[bo_b, sqb], writes=[ssb])
                p.op("act", lambda e, sq=sq, ss=ss, N=N: e.activation(
                    out=sq[:, 0:N], in_=ss[:, 0:N], func=AF.Sqrt, scale=1.0 / 64.0, bias=EPS),
                    reads=[ssb], writes=[sqb])
                p.op("dve", lambda e, sq=sq, N=N: e.reciprocal(out=sq[:, 0:N], in_=sq[:, 0:N]),
                     reads=[sqb], writes=[sqb])
                p.op("dve", lambda e, sq=sq, ps=ps, nt=nt, N=N: e.scalar_tensor_tensor(
                    out=kcT[:, nt * 512:nt * 512 + N], in0=ps[:, 0:N], scalar=kn[:, 0:1], in1=sq[:, 0:N],
                    op0=ALU.mult, op1=ALU.mult), reads=[psb, sqb, kn_b], writes=[kcT_b])
        else:
            for h in range(2):
                for c in range(8):
                    ps, psb = pS.next()
                    p.op("pe", lambda e, ps=ps, h=h, c=c: e.matmul(
                        out=ps[:, 0:64], lhsT=hid[h][:, c * 128:(c + 1) * 128], rhs=w2v[:, :], start=True, stop=True),
                        reads=[hid_b[h], w2v_b], writes=[psb])
                    p.op("act", lambda e, ps=ps, h=h, c=c: e.copy(out=vc[:, c, h, 0:64], in_=ps[:, 0:64]),
                         reads=[psb], writes=[vc_b])
    p.dma("sp", big[:, :], ksel_d[:, :], writes=[big_b])
    ksel, ksel_b = big, big_b

    qr = Ring(ctx, 2, [128, 512], BF16, "q")
    kwr = Ring(ctx, 2, [128, 640], BF16, "kw")
    vwr = Ring(ctx, 2, [128, 5, 2, 65], BF16, "vw")
    ecmp = [ctx.sb([128, 1024], F32) for _ in range(4)]
    ecmp_b = [p.buf() for _ in range(4)]
    cm, cm_b = ctx.sb([128, 1024], F32), p.buf()
    imp, imp_b = ctx.sb([128, 1024], F32), p.buf()
    den, den_b = ctx.sb([128, 8], F32), p.buf()
    pb16 = Ring(ctx, 2, [128, 1024], BF16, "pb16")
    pTs = Ring(ctx, 3, [128, 128], BF16, "pTs")
    sc, sc_b = ctx.sb([128, 256], F32), p.buf()
    sc2, sc2_b = ctx.sb([128, 256], F32), p.buf()
    m8, m8_b = ctx.sb([128, 16], F32), p.buf()
    selm = [ctx.sb([128, 128], BF16) for _ in range(2)]
    selm_b = p.buf()
    for hf_ in range(2):
        p.op("pool", lambda e, hf_=hf_: e.memset(selm[hf_][:, :], 0.0), writes=[selm_b])
    mT = Ring(ctx, 4, [128, 128], BF16, "mT")
    er = Ring(ctx, 5, [128, 512], BF16, "e")
    ptr = Ring(ctx, 5, [128, 4, 128], BF16, "pts")
    mkr = Ring(ctx, 4, [128, 128], F32, "mk")
    fr = Ring(ctx, 2, [128, 8], F32, "f")
    oacc = Ring(ctx, 2, [128, 512], F32, "oacc")

    def evac(ac, acb, j, h, br, o, ob, first):
        f, fb = fr.next()
        p.op("dve", lambda e: e.tensor_scalar(out=f[:, 0:4], in0=ac[:, :, 64], scalar1=1e-30, scalar2=None,
                                              op0=ALU.max), reads=[acb], writes=[fb])
        p.op("dve", lambda e: e.reciprocal(out=f[:, 0:4], in_=f[:, 0:4]), reads=[fb], writes=[fb])
        p.op("dve", lambda e: e.tensor_tensor(out=f[:, 4:8], in0=f[:, 0:4],
                                              in1=gat[:, j, h * 12 + br:h * 12 + 12:3], op=ALU.mult),
             reads=[fb, gat_b], writes=[fb])
        ov = o[:, h * 256:(h + 1) * 256].rearrange("p (g d) -> p g d", g=4)
        fbc = f[:, 4:8].unsqueeze(2).broadcast_to([128, 4, 64])
        if first:
            p.op("dve", lambda e: e.tensor_tensor(out=ov, in0=ac[:, :, 0:64], in1=fbc, op=ALU.mult),
                 reads=[acb, fb], writes=[ob])
        else:
            t, tb = mkr.next()
            for half in range(2):
                tv = t[:, :].rearrange("p (g d) -> p g d", g=2)
                p.op("dve", lambda e, half=half, tv=tv: e.tensor_tensor(
                    out=tv, in0=ac[:, 2 * half:2 * half + 2, 0:64],
                    in1=f[:, 4 + 2 * half:6 + 2 * half].unsqueeze(2).broadcast_to([128, 2, 64]), op=ALU.mult),
                    reads=[acb, fb], writes=[tb])
                p.op("dve", lambda e, half=half, tv=tv: e.tensor_tensor(
                    out=ov[:, 2 * half:2 * half + 2, :], in0=ov[:, 2 * half:2 * half + 2, :], in1=tv, op=ALU.add),
                    reads=[tb, ob], writes=[ob])

    loads = {}

    def get_load(j):
        if j not in loads:
            L = {}
            L["q"], L["qb"] = qr.next()
            p.dma("sp", L["q"][:, :], qT_d[:, j, :], writes=[L["qb"]])
            L["kw"], L["kwb"] = kwr.next()
            p.dma("sp", L["kw"][:, :], kw_d[:, j, :], writes=[L["kwb"]])
            L["vw"], L["vwb"] = vwr.next()
            p.dma("sp", L["vw"][:, :, :, :], vw_d[:, j, :, :, :], writes=[L["vwb"]])
            L["o"], L["ob"] = oacc.next()
            loads[j] = L
        return loads[j]

    mts_of = {}

    def cmp_stage(j, h):
        L = get_load(j)
        q, qb, o, ob = L["q"], L["qb"], L["o"], L["ob"]
        nb = 8 * (j + 1)
        Nv = 32 * (j + 1)
        hs = slice(h * 64, (h + 1) * 64)
        if h == 0:
            p.op("dve", lambda e: e.tensor_scalar(out=cm[:, 0:Nv], in0=iotc[:, 0:Nv], scalar1=thr[:, j:j + 1],
                                                  scalar2=None, op0=ALU.is_le),
                 reads=[iotc_b, thr_b], writes=[cm_b])
        for g in range(4):
            for nt in range((Nv + 511) // 512):
                n0, n1 = nt * 512, min(Nv, nt * 512 + 512)
                ps, psb = pS.next()
                p.op("pe", lambda e, ps=ps, g=g, n0=n0, n1=n1, q=q, hs=hs: e.matmul(
                    out=ps[:, 0:n1 - n0], lhsT=q[hs, g * 128:(g + 1) * 128], rhs=kcT[hs, n0:n1],
                    start=True, stop=True), reads=[qb, kcT_b], writes=[psb])
                p.op("act", lambda e, ps=ps, g=g, n0=n0, n1=n1: e.activation(
                    out=ecmp[g][:, n0:n1], in_=ps[:, 0:n1 - n0], func=AF.Exp), reads=[psb], writes=[ecmp_b[g]])
            p.op("dve", lambda e, g=g, Nv=Nv: e.scalar_tensor_tensor(
                out=ecmp[g][:, 0:Nv], in0=ecmp[g][:, 0:Nv], scalar=1.0, in1=cm[:, 0:Nv], op0=ALU.mult,
                op1=ALU.mult, accum_out=den[:, g:g + 1]), reads=[ecmp_b[g], cm_b], writes=[ecmp_b[g], den_b])
            yield
        p.op("dve", lambda e: e.tensor_scalar(out=den[:, 4:8], in0=den[:, 0:4], scalar1=1e-30, scalar2=None,
                                              op0=ALU.max), reads=[den_b], writes=[den_b])
        p.op("dve", lambda e: e.reciprocal(out=den[:, 4:8], in_=den[:, 4:8]), reads=[den_b], writes=[den_b])
        p.op("dve", lambda e, Nv=Nv: e.tensor_scalar(out=imp[:, 0:Nv], in0=ecmp[0][:, 0:Nv], scalar1=den[:, 4:5],
                                                     scalar2=None, op0=ALU.mult),
             reads=[ecmp_b[0], den_b], writes=[imp_b])
        for g in range(1, 4):
            p.op("dve", lambda e, g=g, Nv=Nv: e.scalar_tensor_tensor(
                out=imp[:, 0:Nv], in0=ecmp[g][:, 0:Nv], scalar=den[:, 4 + g:5 + g], in1=imp[:, 0:Nv],
                op0=ALU.mult, op1=ALU.add), reads=[ecmp_b[g], den_b, imp_b], writes=[imp_b])
        yield
        ac, acb = acc.next()
        nch = (Nv + 127) // 128
        for g in range(4):
            pb, pbb = pb16.next()
            p.op("act", lambda e, pb=pb, g=g, Nv=Nv: e.copy(out=pb[:, 0:Nv], in_=ecmp[g][:, 0:Nv]),
                 reads=[ecmp_b[g]], writes=[pbb])
            for c in range(nch):
                w = min(128, Nv - c * 128)
                pt, ptb = next_pT()
                p.op("pe", lambda e, pt=pt, pb=pb, c=c, w=w: e.transpose(
                    out=pt[0:w, :], in_=pb[:, c * 128:c * 128 + w], identity=ident[:, :]),
                    reads=[pbb, id_b], writes=[ptb])
                ts, tsb = pTs.next()
                p.op("act", lambda e, ts=ts, pt=pt, w=w: e.copy(out=ts[0:w, :], in_=pt[0:w, :]),
                     reads=[ptb], writes=[tsb])
                p.op("pe", lambda e, ac=ac, ts=ts, g=g, c=c, w=w, h=h, nch=nch: e.matmul(
                    out=ac[:, g, 0:65], lhsT=ts[0:w, :], rhs=vc[0:w, c, h, :],
                    start=(g == 0 and c == 0), stop=(g == 3 and c == nch - 1), skip_group_check=True),
                    reads=[tsb, vc_b], writes=[acb])
                yield
        evac(ac, acb, j, h, 0, o, ob, True)
        yield
        p.op("dve", lambda e, nb=nb: e.tensor_reduce(
            out=sc[:, 0:nb], in_=imp[:, 0:4 * nb].rearrange("p (b r) -> p b r", r=4), axis=AX.X, op=ALU.add),
            reads=[imp_b], writes=[sc_b])
        p.op("dve", lambda e, nb=nb: e.tensor_tensor(out=sc[:, 1:nb], in0=sc[:, 1:nb], in1=imp[:, 3:4 * nb - 1:4],
                                                     op=ALU.add), reads=[imp_b, sc_b], writes=[sc_b])
        yield
        f0 = 256 - 8 * j
        p.op("dve", lambda e, nb=nb, f0=f0: e.tensor_tensor(out=sc[:, 0:nb], in0=sc[:, 0:nb],
                                                            in1=FT[:, f0:f0 + nb], op=ALU.add),
             reads=[sc_b, FT_b], writes=[sc_b])
        p.op("dve", lambda e, nb=nb, f0=f0: e.tensor_tensor(out=sc[:, 0:nb], in0=sc[:, 0:nb],
                                                            in1=CAPT[:, f0:f0 + nb], op=ALU.min),
             reads=[sc_b, CAP_b], writes=[sc_b])
        p.op("dve", lambda e: e.memset(sc[:, 0:1], 1e6), reads=[], writes=[sc_b])
        if nb >= 24 and 'tk_sort' not in DBG_SKIP:
            p.op("dve", lambda e, nb=nb: e.max(out=m8[:, 0:8], in_=sc[:, 0:nb]), reads=[sc_b], writes=[m8_b])
            p.op("dve", lambda e, nb=nb: e.match_replace(out=sc2[:, 0:nb], in_to_replace=m8[:, 0:8],
                                                         in_values=sc[:, 0:nb], imm_value=-2e30),
                 reads=[sc_b, m8_b], writes=[sc2_b])
            p.op("dve", lambda e, nb=nb: e.max(out=m8[:, 8:16], in_=sc2[:, 0:nb]), reads=[sc2_b], writes=[m8_b])
            p.op("dve", lambda e: e.tensor_scalar(out=m8[:, 0:1], in0=m8[:, 15:16], scalar1=-1.0, scalar2=None,
                                                  op0=ALU.max), reads=[m8_b], writes=[m8_b])
            for hf in range((nb + 127) // 128):
                c0_, c1_ = hf * 128, min(nb, hf * 128 + 128)
                p.op("dve", lambda e, hf=hf, c0_=c0_, c1_=c1_: e.tensor_scalar(
                    out=selm[hf][:, 0:c1_ - c0_], in0=sc[:, c0_:c1_], scalar1=m8[:, 0:1], scalar2=None,
                    op0=ALU.is_ge), reads=[sc_b, m8_b], writes=[selm_b])
        else:
            p.op("dve", lambda e, nb=nb: e.tensor_scalar(out=selm[0][:, 0:nb], in0=sc[:, 0:nb], scalar1=-1.0,
                                                         scalar2=None, op0=ALU.is_ge),
                 reads=[sc_b], writes=[selm_b])
        yield
        mts = []
        for hf in range(0 if 'tk_tr' in DBG_SKIP else (nb + 127) // 128):
            mt, mtb = mT.next()
            mts.append((mt, mtb))
            pt, ptb = next_pT()
            p.op("pe", lambda e, pt=pt, hf=hf: e.transpose(
                out=pt, in_=selm[hf][:, :], identity=ident[:, :]),
                reads=[selm_b, id_b], writes=[ptb])
            p.op("act", lambda e, mt=mt, pt=pt: e.copy(out=mt[:, :], in_=pt), reads=[ptb], writes=[mtb])
        mts_of[(j, h)] = mts

    def pairs_stage(j, h, advance, drain):
        L = get_load(j)
        q, qb, o, ob = L["q"], L["qb"], L["o"], L["ob"]
        kw, kwb, vw, vwb = L["kw"], L["kwb"], L["vw"], L["vwb"]
        nsel = 4 * j + 4
        hs = slice(h * 64, (h + 1) * 64)
        mts = mts_of.pop((j, h))
        qv = q[hs, :]
        acs, acsb = acc.next()
        wacc = {}
        descs = []
        for kc in range(nsel):
            descs.append(dict(k=ksel[hs, kc * 128:(kc + 1) * 128], kb=ksel_b,
                              mask=("selc", kc) if kc >= 4 * j else ("sel", kc), v=vsel[:, kc, h, :], vb=vsel_b,
                              ac=acs, acb=acsb, first=(kc == 0), last=(kc == nsel - 1), br=1))
        if True:
            for m in range(5):
                descs.append(dict(k=kw[hs, m * 128:(m + 1) * 128], kb=kwb,
                                  mask=("win", 0) if m == 0 else (("win", 1) if m == 4 else None),
                                  v=vw[:, m, h, :], vb=vwb, ac=None, acb=None, first=(m == 0), last=(m == 4), br=2))

        def stage1a(dsc):
            ps, psb = pS.next()
            p.op("pe", lambda e: e.matmul(out=ps[:, :], lhsT=dsc["k"], rhs=qv, start=True, stop=True),
                 reads=[dsc["kb"], qb], writes=[psb])
            et, etb = er.next()
            p.op("act", lambda e: e.activation(out=et[:, :], in_=ps[:, :], func=AF.Exp),
                 reads=[psb], writes=[etb])
            return et, etb

        def stage1b(dsc, et, etb):
            mk_ = dsc["mask"]
            ev = et[:, :].rearrange("p (g q) -> p g q", g=4)
            if mk_ is None:
                return ev, etb
            if mk_[0] in ("sel", "selc"):
                kc = mk_[1]
                mt, mtb = mts[kc // 64]
                mx, mxb = next_pmx()
                p.op("pe", lambda e: e.matmul(
                    out=mx, lhsT=Ebig[:, 128 * (kc % 64):128 * (kc % 64) + 128], rhs=mt[:, :],
                    start=True, stop=True), reads=[eb_b, mtb], writes=[mxb])
            pt_, ptb_ = ptr.next()
            if mk_[0] == "selc":
                mk, mkb = mkr.next()
                p.op("dve", lambda e: e.tensor_tensor(out=mk[:, :], in0=mx, in1=c4[:, mk_[1] - 4 * j, :],
                                                      op=ALU.mult), reads=[mxb, c4_b], writes=[mkb])
                p.op("dve", lambda e: e.tensor_tensor(
                    out=pt_[:, :, :], in0=ev, in1=mk[:, :].unsqueeze(1).broadcast_to([128, 4, 128]), op=ALU.mult),
                    reads=[etb, mkb], writes=[ptb_])
            elif mk_[0] == "sel":
                p.op("dve", lambda e: e.tensor_tensor(
                    out=pt_[:, :, :], in0=ev, in1=mx.unsqueeze(1).broadcast_to([128, 4, 128]), op=ALU.mult),
                    reads=[etb, mxb], writes=[ptb_])
            else:
                wi = mk_[1]
                p.op("dve", lambda e: e.tensor_tensor(
                    out=pt_[:, :, :], in0=ev, in1=wm[:, wi, :].unsqueeze(1).broadcast_to([128, 4, 128]),
                    op=ALU.mult), reads=[etb, wm_b], writes=[ptb_])
            return pt_, ptb_

        def stage2(dsc, src, srcb):
            if dsc["br"] == 2:
                if "ac" not in wacc:
                    drain()
                    wacc["ac"], wacc["acb"] = acc.next()
                ac_, acb_ = wacc["ac"], wacc["acb"]
            else:
                ac_, acb_ = dsc["ac"], dsc["acb"]
            for g in range(4):
                p.op("pe", lambda e, g=g: e.matmul(
                    out=ac_[:, g, 0:65], lhsT=src[:, g, :], rhs=dsc["v"],
                    start=(g == 0 and dsc["first"]), stop=(g == 3 and dsc["last"]), skip_group_check=True),
                    reads=[srcb, dsc["vb"]], writes=[acb_])
            if dsc["last"]:
                evac(ac_, acb_, j, h, dsc["br"], o, ob, False)

        nd = len(descs)
        st_a = {}
        st_b = {}
        for t in range(nd + 2):
            if t < nd:
                st_a[t] = stage1a(descs[t])
            if t < nsel:
                advance()
            if 0 <= t - 1 < nd:
                st_b[t - 1] = stage1b(descs[t - 1], *st_a.pop(t - 1))
            if 0 <= t - 2 < nd:
                stage2(descs[t - 2], *st_b.pop(t - 2))

    items = [(j, h) for j in range(J0, NJ) for h in range(2)]
    for _ in cmp_stage(*items[0]):
        pass
    for idx, (j, h) in enumerate(items):
        nxt = cmp_stage(*items[idx + 1]) if idx + 1 < len(items) else iter(())

        def advance(nxt=nxt):
            next(nxt, None)

        def drain(nxt=nxt):
            for _ in nxt:
                pass

        pairs_stage(j, h, advance, drain)
        drain()
        if h == 1:
            L = loads.pop(j)
            p.dma("pool", yo_d[j, :, :], L["o"][:, :], reads=[L["ob"]], is_output=True)
    return ctx


def battn_consts(cc):
    qi = np.arange(128)
    n = np.arange(1024)
    thr = np.broadcast_to((128.0 * (4 * np.arange(32) + cc))[None, :], (128, 32)).astype(np.float32)
    iotaC = (16.0 * n[None, :] + 31.0 - qi[:, None]).astype(np.float32)
    rp = np.arange(512) - 256
    hi = (qi >= 64).astype(np.int64)[:, None]
    forced = (rp[None, :] == 2 * cc + hi) | (rp[None, :] == 2 * cc + hi - 1)
    valid = rp[None, :] <= 2 * cc + hi
    FT = np.where(forced, 1e6, 0.0).astype(np.float32)
    CAPT = np.where(valid, 3e6, NEG).astype(np.float32)
    k = np.arange(128)
    c4 = np.stack([(k[:, None] - qi[None, :] <= 128 * (cc - dd)) for dd in range(4)], axis=1).astype(np.float32)
    wm = np.stack([k[:, None] > qi[None, :], k[:, None] <= qi[None, :]], axis=1).astype(np.float32)
    Ebig = (np.arange(8192)[None, :] // 64 == np.arange(128)[:, None]).astype(np.float32).astype(NPBF)
    return dict(thr=np.ascontiguousarray(thr), iotaC=iotaC, FT=FT, CAPT=CAPT, causal4=c4, wmask=wm, Ebig=Ebig,
                **consts_a())


def battn_inputs(inp, l, fa_b, gat_b, cc, NJ=32):
    blocks = 4 * np.arange(NJ) + cc
    q = fa_b[0:512].reshape(2, 4, 64, S // 128, 128)[:, :, :, blocks, :]
    qT = np.ascontiguousarray(q.transpose(0, 2, 3, 1, 4).reshape(128, NJ, 512))
    vs = fa_b[896:1024].reshape(2, 64, 128, 128).transpose(3, 2, 0, 1)
    vsel = np.ones((128, 128, 2, 65), NPBF)
    vsel[..., 0:64] = vs
    kwp = np.concatenate([np.zeros((128, 512), NPBF), fa_b[1024:1152]], axis=1)
    vwp = np.concatenate([np.zeros((128, 512), NPBF), fa_b[1152:1280]], axis=1)
    valid = np.concatenate([np.zeros(512, NPBF), np.ones(S, NPBF)])
    kw = np.stack([kwp[:, 128 * i:128 * i + 640] for i in blocks], axis=1)
    vw = np.zeros((128, NJ, 5, 2, 65), NPBF)
    for jj, i in enumerate(blocks):
        seg = vwp[:, 128 * i:128 * i + 640].reshape(2, 64, 5, 128)
        vw[:, jj, :, :, 0:64] = seg.transpose(3, 2, 0, 1)
        vw[:, jj, :, :, 64] = valid[128 * i:128 * i + 640].reshape(5, 128).T[:, :, None]
    gates = np.ascontiguousarray(gat_b.reshape(24, S // 128, 128)[:, blocks, :].transpose(2, 1, 0))
    w1 = inp["cmp_w1"][l]
    w1rep = np.stack([np.tile(w1[kv].reshape(32, 64, 128).transpose(1, 0, 2).reshape(64, 4096), (2, 1))
                      for kv in range(2)], axis=0)
    posT = np.ascontiguousarray(inp["cmp_pos"][l].transpose(2, 0, 1))
    w2 = inp["cmp_w2"][l]
    w2h = np.zeros((128, 2, 128), np.float32)
    w2h[:, 0, 0:64] = w2[0]
    w2h[:, 1, 64:128] = w2[0]
    return dict(qT=qT, kcin=np.ascontiguousarray(fa_b[512:640]), vcin=np.ascontiguousarray(fa_b[640:768]),
                kselT=np.ascontiguousarray(fa_b[768:896]), vsel=vsel, kw=np.ascontiguousarray(kw), vw=vw,
                gates=gates.astype(np.float32), w1rep=np.ascontiguousarray(w1rep.astype(np.float32)), posT=posT,
                w2h=w2h.reshape(128, 256), w2v=np.ascontiguousarray(w2[1]),
                kn0=np.tile(inp["k_norm"][l][0], 2).reshape(128, 1).astype(np.float32), **battn_consts(cc))


def run_phase_battn(inp, l, fa_seq, gat_seq, NJ=32):
    ctx = build_phase_battn(NJ)
    in_maps = [battn_inputs(inp, l, fa_seq[c // 4], gat_seq[c // 4], c % 4, NJ) for c in range(NCORES)]
    res = _run(ctx, in_maps)
    y = np.zeros((NB, S // 128, 128, 512), np.float32)
    for c in range(NCORES):
        y[c // 4, 4 * np.arange(NJ) + c % 4] = np.asarray(res[c]["yatt"])
    return y.reshape(NB, S, 512)


def kernel(**inputs):
    inp = {k: np.asarray(v) for k, v in inputs.items()}
    x = np.ascontiguousarray(inp["x"], dtype=np.float32).reshape(NB * S, D)
    xs = [x[c * TA:(c + 1) * TA] for c in range(NCORES)]
    cBa = ATTN_SPLITS
    ca = consts_a()
    for l in range(DEPTH):
        last = l == DEPTH - 1
        gq = np.stack([np.tile(inp["q_norm"][l], 2), np.tile(inp["k_norm"][l][1], 2),
                       np.tile(inp["k_norm"][l][2], 2)], axis=1).astype(np.float32)
        common = dict(w=np.ascontiguousarray(inp["w_in"][l]),
                      gcol=np.ascontiguousarray(inp["attn_norm"][l].reshape(8, 128).T), gq=np.ascontiguousarray(gq), **ca)
        ra = _run(build_phase_a(), [dict(x=np.ascontiguousarray(xs[c]), **common) for c in range(NCORES)])
        fa_seq = [np.concatenate([np.asarray(ra[4 * b + q]["fa"]) for q in range(4)], axis=1) for b in range(NB)]
        if fa_seq[0].dtype != NPBF:
            fa_seq = [a.astype(NPBF) for a in fa_seq]
        gat_seq = [np.concatenate([np.asarray(ra[4 * b + q]["gat"]) for q in range(4)], axis=1) for b in range(NB)]
        fl_seq = [np.concatenate([np.asarray(ra[4 * b + q]["fl"]) for q in range(4)], axis=1) for b in range(NB)]
        del ra
        y_attn = np.zeros((NB, S // 128, 128, 512), np.float32)
        full = [battn_inputs(inp, l, fa_seq[c // 4], gat_seq[c // 4], c % 4) for c in range(NCORES)]
        for (j0, j1) in cBa:
            maps = []
            for c in range(NCORES):
                m = dict(full[c])
                for k in ("qT", "kw", "vw", "gates"):
                    m[k] = np.ascontiguousarray(m[k][:, 0:j1])
                maps.append(m)
            rb = _run(build_phase_battn(j1, j0), maps)
            for c in range(NCORES):
                y_attn[c // 4, 4 * np.arange(j0, j1) + c % 4] = np.asarray(rb[c]["yatt"])[j0:j1]
            del rb, maps
        y_attn = y_attn.reshape(NB * S, 512)
        del full, fa_seq, gat_seq
        maps = []
        for c in range(NCORES):
            b, cc = c // 4, c % 4
            sl = slice(64 * cc, 64 * cc + 64)
            maps.append(dict(lx=np.ascontiguousarray(fl_seq[b][0:256][sl]), lg=np.ascontiguousarray(fl_seq[b][256:512][sl]),
                             cv=np.ascontiguousarray(fl_seq[b][512:768][sl]), **bseq_params(inp, l, cc)))
        rs = _run(build_phase_bseq(), maps)
        y_lru = np.concatenate([np.concatenate([np.asarray(rs[4 * b + q]["ylT"]) for q in range(4)], axis=0).T
                                for b in range(NB)], axis=0)
        y_cv = np.concatenate([np.concatenate([np.asarray(rs[4 * b + q]["ycT"]) for q in range(4)], axis=0).T
                               for b in range(NB)], axis=0)
        del rs, fl_seq, maps
        lnrep = np.ascontiguousarray(np.broadcast_to(
            np.concatenate([inp["cv_ln_g"][l], inp["cv_ln_b"][l]])[None, :], (128, 512))).astype(np.float32)
        common = dict(w=np.ascontiguousarray(inp["w_out"][l]),
                      gcol=np.ascontiguousarray(inp["out_norm"][l].reshape(8, 128).T), lnrep=lnrep, ident=ca["ident"])
        r1 = _run(build_phase_c1(), [dict(x=np.ascontiguousarray(xs[c]), ya=np.ascontiguousarray(y_attn[c * TA:(c + 1) * TA]),
                             yl=np.ascontiguousarray(y_lru[c * TA:(c + 1) * TA]),
                             yc=np.ascontiguousarray(y_cv[c * TA:(c + 1) * TA]), **common) for c in range(NCORES)])
        xs = [np.asarray(r["xo"]) for r in r1]
        del r1, y_attn, y_lru, y_cv
        common = dict(w1=np.ascontiguousarray(inp["mlp_w1"][l]), w2=np.ascontiguousarray(inp["mlp_w2"][l]),
                      gcol=np.ascontiguousarray(inp["mlp_norm"][l].reshape(8, 128).T), ident=ca["ident"])
        r2 = _run(build_phase_c2(), [dict(x=np.ascontiguousarray(xs[c]), **common) for c in range(NCORES)])
        xs = [np.asarray(r["xo"]) for r in r2]
        del r2
    return np.concatenate(xs, axis=0).reshape(NB, S, D).astype(np.float32)
```
